# Optimizing a Trainium2 kernel written in Bass

```python
import jax, jax.numpy as jnp
from jax import lax
import numpy as np

D_MODEL = 2048
BATCH = 4
SEQ = 2048
DEPTH = 4

N_HEADS = 8
HEAD_DIM = 128
ATTN_WIDTH = N_HEADS * HEAD_DIM
CONV_WIDTH = D_MODEL // 2
CONV_KERNEL = 31
D_FF = 5632
N_EXPERTS = 8
TOP_K = 2
Q_BLOCK = 128
LN_EPS = 1e-5
DEEPNORM_ALPHA = (2 * DEPTH) ** 0.25
DEEPNORM_BETA = (8 * DEPTH) ** -0.25
FORGET_BIAS_SHIFT = 3.0
N_DENSE = (DEPTH + 1) // 2
N_MOE = DEPTH // 2

_OFF_Q = 2 * CONV_WIDTH
_OFF_K = _OFF_Q + ATTN_WIDTH
_OFF_V = _OFF_K + ATTN_WIDTH
_OFF_F = _OFF_V + ATTN_WIDTH
_OFF_G = _OFF_F + N_HEADS
IN_COLS = _OFF_G + 2 * D_MODEL

kernel_name = "hybrid_conformer_fox_moe_deepnorm"


def layer_norm(x, g, b):
    xf = x.astype(jnp.float32)
    mu = jnp.mean(xf, axis=-1, keepdims=True)
    var = jnp.mean(jnp.square(xf - mu), axis=-1, keepdims=True)
    y = (xf - mu) * lax.rsqrt(var + LN_EPS)
    return (y * g.astype(jnp.float32) + b.astype(jnp.float32)).astype(x.dtype)


def conv_branch(u_glu, dw_w, dw_b, ln_g, ln_b, w_out, b_out):
    a, gate = jnp.split(u_glu, 2, axis=-1)
    u = a * jax.nn.sigmoid(gate)
    y = lax.conv_general_dilated(
        u, dw_w[:, None, :], window_strides=(1,),
        padding=[(CONV_KERNEL - 1, 0)],
        dimension_numbers=("NWC", "WIO", "NWC"),
        feature_group_count=CONV_WIDTH) + dw_b
    y = jax.nn.silu(layer_norm(y, ln_g, ln_b))
    return y @ w_out + b_out


def forgetting_attention(q, k, v, f_logit):
    B, S = q.shape[0], q.shape[1]
    nb = S // Q_BLOCK
    cum = jnp.cumsum(jax.nn.log_sigmoid(f_logit.astype(jnp.float32)), axis=1)
    cum = jnp.transpose(cum, (0, 2, 1))
    qh = jnp.transpose(q, (0, 2, 1, 3)) * (HEAD_DIM ** -0.5)
    kh = jnp.transpose(k, (0, 2, 1, 3))
    vh = jnp.transpose(v, (0, 2, 1, 3))
    q_blocks = qh.reshape(B, N_HEADS, nb, Q_BLOCK, HEAD_DIM).transpose(2, 0, 1, 3, 4)
    c_blocks = cum.reshape(B, N_HEADS, nb, Q_BLOCK).transpose(2, 0, 1, 3)
    k_pos = jnp.arange(S)

    def one_block(args):
        qb, cb, i = args
        s = jnp.einsum("bhqd,bhkd->bhqk", qb, kh).astype(jnp.float32)
        s = s + cb[..., None] - cum[:, :, None, :]
        q_pos = i * Q_BLOCK + jnp.arange(Q_BLOCK)
        s = jnp.where(k_pos[None, :] <= q_pos[:, None], s, -jnp.inf)
        p = jax.nn.softmax(s, axis=-1).astype(vh.dtype)
        return jnp.einsum("bhqk,bhkd->bhqd", p, vh)

    out = lax.map(one_block, (q_blocks, c_blocks, jnp.arange(nb)))
    return out.transpose(1, 0, 3, 2, 4).reshape(B, S, ATTN_WIDTH)


def mixer(x, w_in, b_in, conv_w, conv_b, conv_ln_g, conv_ln_b, w_conv_out, b_conv_out,
          w_attn_out, w_o):
    B, S, _ = x.shape
    p = x @ w_in + b_in
    q = p[..., _OFF_Q:_OFF_K].reshape(B, S, N_HEADS, HEAD_DIM)
    k = p[..., _OFF_K:_OFF_V].reshape(B, S, N_HEADS, HEAD_DIM)
    v = p[..., _OFF_V:_OFF_F].reshape(B, S, N_HEADS, HEAD_DIM)
    f_logit = p[..., _OFF_F:_OFF_G]
    g_conv = jax.nn.sigmoid(p[..., _OFF_G:_OFF_G + D_MODEL])
    g_attn = jax.nn.sigmoid(p[..., _OFF_G + D_MODEL:])
    y_conv = conv_branch(p[..., :_OFF_Q], conv_w, conv_b, conv_ln_g, conv_ln_b,
                         w_conv_out, b_conv_out)
    y_attn = forgetting_attention(q, k, v, f_logit) @ w_attn_out
    return (g_conv * y_conv + g_attn * y_attn) @ w_o


def swiglu(x, wg, wu, wd):
    return (jax.nn.silu(x @ wg) * (x @ wu)) @ wd


def moe_swiglu(x, w_router, b_router, wg, wu, wd):
    B, S, D = x.shape
    t = x.reshape(-1, D)
    logits = (t @ w_router).astype(jnp.float32) + b_router.astype(jnp.float32)
    top_v, top_i = lax.top_k(logits, TOP_K)
    top_w = jax.nn.softmax(top_v, axis=-1)
    comb = jnp.sum(jax.nn.one_hot(top_i, N_EXPERTS, dtype=jnp.float32) * top_w[..., None], axis=1)
    comb = comb.astype(x.dtype)
    y = jnp.zeros_like(t)
    for e in range(N_EXPERTS):
        y = y + comb[:, e:e + 1] * swiglu(t, wg[e], wu[e], wd[e])
    return y.reshape(B, S, D)


def _normal(key, shape, scale):
    return jax.random.normal(key, shape, jnp.float32) * scale


def setup_inputs(seed: int = 0) -> dict:
    key = jax.random.key(seed)
    ks = jax.random.split(key, 24)
    D, Cc, F, E = D_MODEL, CONV_WIDTH, D_FF, N_EXPERTS
    x = _normal(ks[0], (BATCH, SEQ, D), 1.0)
    w_in = _normal(ks[1], (DEPTH, D, IN_COLS), D ** -0.5)
    w_in = w_in.at[:, :, _OFF_V:_OFF_F].multiply(DEEPNORM_BETA)
    b_in = _normal(ks[2], (DEPTH, IN_COLS), 0.02).at[:, _OFF_F:_OFF_G].add(FORGET_BIAS_SHIFT)
    conv_w = _normal(ks[3], (DEPTH, CONV_KERNEL, Cc), CONV_KERNEL ** -0.5)
    conv_b = _normal(ks[4], (DEPTH, Cc), 0.02)
    conv_ln_g = 1.0 + _normal(ks[5], (DEPTH, Cc), 0.02)
    conv_ln_b = _normal(ks[6], (DEPTH, Cc), 0.02)
    w_conv_out = _normal(ks[7], (DEPTH, Cc, D), Cc ** -0.5)
    b_conv_out = _normal(ks[8], (DEPTH, D), 0.02)
    w_attn_out = _normal(ks[9], (DEPTH, ATTN_WIDTH, D), ATTN_WIDTH ** -0.5)
    w_o = _normal(ks[10], (DEPTH, D, D), DEEPNORM_BETA * D ** -0.5)
    ln1_g = 1.0 + _normal(ks[11], (DEPTH, D), 0.02)
    ln1_b = _normal(ks[12], (DEPTH, D), 0.02)
    ffn_wg = _normal(ks[13], (N_DENSE, D, F), D ** -0.5)
    ffn_wu = _normal(ks[14], (N_DENSE, D, F), D ** -0.5)
    ffn_wd = _normal(ks[15], (N_DENSE, F, D), DEEPNORM_BETA * F ** -0.5)
    router_w = _normal(ks[16], (N_MOE, D, E), D ** -0.5)
    router_b = _normal(ks[17], (N_MOE, E), 0.01)
    exp_wg = _normal(ks[18], (N_MOE, E, D, F), D ** -0.5)
    exp_wu = _normal(ks[19], (N_MOE, E, D, F), D ** -0.5)
    exp_wd = _normal(ks[20], (N_MOE, E, F, D), DEEPNORM_BETA * F ** -0.5)
    ln2_g = 1.0 + _normal(ks[21], (DEPTH, D), 0.02)
    ln2_b = _normal(ks[22], (DEPTH, D), 0.02)
    return {"x": x, "w_in": w_in, "b_in": b_in, "conv_w": conv_w, "conv_b": conv_b,
            "conv_ln_g": conv_ln_g, "conv_ln_b": conv_ln_b, "w_conv_out": w_conv_out,
            "b_conv_out": b_conv_out, "w_attn_out": w_attn_out, "w_o": w_o,
            "ln1_g": ln1_g, "ln1_b": ln1_b, "ffn_wg": ffn_wg, "ffn_wu": ffn_wu, "ffn_wd": ffn_wd,
            "router_w": router_w, "router_b": router_b, "exp_wg": exp_wg, "exp_wu": exp_wu,
            "exp_wd": exp_wd, "ln2_g": ln2_g, "ln2_b": ln2_b}


def reference(x, w_in, b_in, conv_w, conv_b, conv_ln_g, conv_ln_b, w_conv_out, b_conv_out,
              w_attn_out, w_o, ln1_g, ln1_b, ffn_wg, ffn_wu, ffn_wd, router_w, router_b,
              exp_wg, exp_wu, exp_wd, ln2_g, ln2_b):
    for l in range(DEPTH):
        h = mixer(x, w_in[l], b_in[l], conv_w[l], conv_b[l], conv_ln_g[l], conv_ln_b[l],
                  w_conv_out[l], b_conv_out[l], w_attn_out[l], w_o[l])
        x = layer_norm(DEEPNORM_ALPHA * x + h, ln1_g[l], ln1_b[l])
        j = l // 2
        if l % 2 == 0:
            h = swiglu(x, ffn_wg[j], ffn_wu[j], ffn_wd[j])
        else:
            h = moe_swiglu(x, router_w[j], router_b[j], exp_wg[j], exp_wu[j], exp_wd[j])
        x = layer_norm(DEEPNORM_ALPHA * x + h, ln2_g[l], ln2_b[l])
    return x
```

```python
import numpy as np
import ml_dtypes
import concourse.bass as bass
import concourse.mybir as mybir
from concourse.bass_utils import run_bass_kernel_spmd

F32 = mybir.dt.float32
BF16 = mybir.dt.bfloat16
AF = mybir.ActivationFunctionType
ALU = mybir.AluOpType

D_MODEL = 2048
BATCH = 4
SEQ = 2048
DEPTH = 4
N_HEADS = 8
HEAD_DIM = 128
CONV_K = 31
D_FF = 5632
N_EXPERTS = 8
LN_EPS = 1e-5
ALPHA = (2 * DEPTH) ** 0.25
NCORES = 8


class Sched:
    ENGS = ("tensor", "scalar", "vector", "gpsimd", "sync")
    NSTREAM = 8

    def __init__(self):
        self.ops = {e: [] for e in self.ENGS}
        self.cnt = {}
        self.last_w = {}
        self.readers = {}
        self.waited = {e: {} for e in self.ENGS}
        self.stream_rr = 0
        self.out_dmas = []

    def _deps(self, reads, writes):
        deps = []
        for b in reads:
            if b in self.last_w:
                deps.append(self.last_w[b])
        for b in writes:
            if b in self.last_w:
                deps.append(self.last_w[b])
            deps.extend(self.readers.get(b, ()))
        return deps

    def _commit(self, tok, reads, writes):
        for b in reads:
            self.readers.setdefault(b, []).append(tok)
        for b in writes:
            self.last_w[b] = tok
            self.readers[b] = []

    def _filter(self, eng, deps):
        best = {}
        for s, v in deps:
            if v > best.get(s, 0):
                best[s] = v
        out = []
        w = self.waited[eng]
        for s, v in best.items():
            if w.get(s, 0) < v:
                w[s] = v
                out.append((s, v))
        return out

    def op(self, eng, fn, reads=(), writes=()):
        sem = "c_" + eng
        deps = self._deps(reads, writes)
        val = self.cnt.get(sem, 0) + 1
        self.cnt[sem] = val
        self.ops[eng].append((self._filter(eng, deps), fn, sem, 1))
        self._commit((sem, val), reads, writes)

    def dma(self, fn, reads=(), writes=(), eng="sync", is_out=False):
        st = self.stream_rr
        self.stream_rr = (st + 1) % self.NSTREAM
        sem = "d_%d" % st
        deps = self._deps(reads, writes)
        prev = self.cnt.get(sem, 0)
        if prev:
            deps.append((sem, prev))
        val = prev + 16
        self.cnt[sem] = val
        self.ops[eng].append((self._filter(eng, deps), fn, sem, 16))
        self._commit((sem, val), reads, writes)

    def emit(self, nc, stack):
        names = ["c_" + e for e in self.ENGS if e != "sync"] + ["d_%d" % i for i in range(self.NSTREAM)]
        sems = {n: stack.enter_context(nc.semaphore(n)) for n in names}
        finals = [(s, v) for s, v in self.cnt.items() if v > 0]
        block = stack.enter_context(nc.Block())
        ops = self.ops

        def run(eng_obj, lst, final=False):
            for waits, fn, sem, inc in lst:
                for s, v in waits:
                    eng_obj.wait_ge(sems[s], v)
                ins = fn(eng_obj)
                ins.then_inc(sems[sem], inc)
            if final:
                for s, v in finals:
                    eng_obj.wait_ge(sems[s], v)

        @block.tensor
        def _(e):
            run(e, ops["tensor"])

        @block.scalar
        def _(e):
            run(e, ops["scalar"])

        @block.vector
        def _(e):
            run(e, ops["vector"])

        @block.gpsimd
        def _(e):
            run(e, ops["gpsimd"])

        @block.sync
        def _(e):
            run(e, ops["sync"], final=True)


def _chunks(n):
    out = []
    o = 0
    while o < n:
        s = min(128, n - o)
        out.append((o, s))
        o += s
    return out


def _ln_feature_major(S, nc, v, nk, ntok, gbt, goff, boff, out_f32, out_bf, scr, ps, ones_b, tag, ps_keys=None):
    pk0, pk1 = ps_keys if ps_keys else ((tag, 'ps0'), (tag, 'ps1'))
    nfeat = float(nk * 128)
    for g0 in range(0, ntok, 512):
        n = min(512, ntok - g0)
        sl = slice(g0, g0 + n)
        for k in range(nk):
            S.op("vector", lambda e, k=k, n=n, sl=sl: e.tensor_copy(out=scr["b1"][:, 0:n], in_=v[:, k, sl]),
                 reads=[(tag, "v", k)], writes=[(tag, "b1")])
            S.op("scalar", lambda e, k=k, n=n, sl=sl: e.activation(out=scr["b2"][:, 0:n], in_=v[:, k, sl], func=AF.Square),
                 reads=[(tag, "v", k)], writes=[(tag, "b2")])
            S.op("tensor", lambda e, k=k, n=n, sl=sl: e.matmul(ps[0][:, 0:n], lhsT=ones_b[:, :], rhs=scr["b1"][:, 0:n],
                                                  start=(k == 0), stop=(k == nk - 1)),
                 reads=[(tag, "b1"), (tag, "ones")], writes=[pk0])
            S.op("tensor", lambda e, k=k, n=n, sl=sl: e.matmul(ps[1][:, 0:n], lhsT=ones_b[:, :], rhs=scr["b2"][:, 0:n],
                                                  start=(k == 0), stop=(k == nk - 1)),
                 reads=[(tag, "b2"), (tag, "ones")], writes=[pk1])
        S.op("scalar", lambda e, n=n, sl=sl: e.activation(out=scr["mean"][:, 0:n], in_=ps[0][:, 0:n], func=AF.Copy, scale=1.0 / nfeat),
             reads=[pk0], writes=[(tag, "mean")])
        S.op("vector", lambda e, n=n, sl=sl: e.tensor_tensor(out=scr["t"][:, 0:n], in0=scr["mean"][:, 0:n], in1=scr["mean"][:, 0:n], op=ALU.mult),
             reads=[(tag, "mean")], writes=[(tag, "t")])
        S.op("vector", lambda e, n=n, sl=sl: e.scalar_tensor_tensor(out=scr["rstd"][:, 0:n], in0=ps[1][:, 0:n], scalar=1.0 / nfeat,
                                                        in1=scr["t"][:, 0:n], op0=ALU.mult, op1=ALU.subtract),
             reads=[pk1, (tag, "t")], writes=[(tag, "rstd")])
        S.op("vector", lambda e, n=n, sl=sl: e.tensor_scalar(out=scr["rstd"][:, 0:n], in0=scr["rstd"][:, 0:n], scalar1=0.0, scalar2=LN_EPS,
                                                 op0=ALU.max, op1=ALU.add),
             reads=[(tag, "rstd")], writes=[(tag, "rstd")])
        S.op("scalar", lambda e, n=n, sl=sl: e.activation(out=scr["rstd"][:, 0:n], in_=scr["rstd"][:, 0:n], func=AF.Sqrt),
             reads=[(tag, "rstd")], writes=[(tag, "rstd")])
        S.op("vector", lambda e, n=n, sl=sl: e.reciprocal(out=scr["rstd"][:, 0:n], in_=scr["rstd"][:, 0:n]),
             reads=[(tag, "rstd")], writes=[(tag, "rstd")])
        for k in range(nk):
            S.op("vector", lambda e, k=k, n=n, sl=sl: e.tensor_tensor(out=scr["t"][:, 0:n], in0=v[:, k, sl], in1=scr["mean"][:, 0:n], op=ALU.subtract),
                 reads=[(tag, "v", k), (tag, "mean")], writes=[(tag, "t")])
            S.op("vector", lambda e, k=k, n=n, sl=sl: e.tensor_tensor(out=scr["t"][:, 0:n], in0=scr["t"][:, 0:n], in1=scr["rstd"][:, 0:n], op=ALU.mult),
                 reads=[(tag, "t"), (tag, "rstd")], writes=[(tag, "t")])
            if out_f32 is not None:
                S.op("scalar", lambda e, k=k, n=n, sl=sl: e.activation(out=out_f32[:, k, sl], in_=scr["t"][:, 0:n], func=AF.Identity,
                                                          scale=gbt[:, goff + k:goff + k + 1], bias=gbt[:, boff + k:boff + k + 1]),
                     reads=[(tag, "t"), (tag, "gb")], writes=[(tag, "of", k)])
            if out_bf is not None:
                S.op("scalar", lambda e, k=k, n=n, sl=sl: e.activation(out=out_bf[:, k, sl], in_=scr["t"][:, 0:n], func=AF.Identity,
                                                          scale=gbt[:, goff + k:goff + k + 1], bias=gbt[:, boff + k:boff + k + 1]),
                     reads=[(tag, "t"), (tag, "gb")], writes=[(tag, "ob", k)])


def build_ffn(D, T, FL, TB=512):
    from contextlib import ExitStack
    KD = D // 128
    fch = _chunks(FL)
    NF = len(fch)
    nc = bass.Bass("TRN2", target_bir_lowering=False)
    xT = nc.dram_tensor("xT", [D, T], BF16, kind="ExternalInput").ap()
    comb = nc.dram_tensor("comb", [1, T], F32, kind="ExternalInput").ap()
    wg = nc.dram_tensor("wg", [D, FL], F32, kind="ExternalInput").ap()
    wu = nc.dram_tensor("wu", [D, FL], F32, kind="ExternalInput").ap()
    wd = nc.dram_tensor("wd", [FL, D], F32, kind="ExternalInput").ap()
    yT = nc.dram_tensor("yT", [D, T], F32, kind="ExternalOutput").ap()
    S = Sched()
    with ExitStack() as st:
        sb = lambda name, shape, dt: st.enter_context(nc.sbuf_tensor(name, shape, dt))
        xb = sb("xb", [128, KD, TB], BF16)
        hT = sb("hT", [128, NF, TB], BF16)
        cb1 = sb("cb1", [1, TB], F32)
        ones1 = sb("ones1", [1, 128], F32)
        comb_bc = sb("comb_bc", [128, TB], F32)
        wgf = [sb("wgf%d" % i, [128, KD, 128], F32) for i in range(2)]
        wuf = [sb("wuf%d" % i, [128, KD, 128], F32) for i in range(2)]
        wgb = [sb("wgb%d" % i, [128, KD, 128], BF16) for i in range(2)]
        wub = [sb("wub%d" % i, [128, KD, 128], BF16) for i in range(2)]
        wdf = [sb("wdf%d" % i, [128, NF, 128], F32) for i in range(2)]
        wdb = [sb("wdb%d" % i, [128, NF, 128], BF16) for i in range(2)]
        sg = [sb("sg%d" % i, [128, TB], F32) for i in range(2)]
        yo = [sb("yo%d" % i, [128, TB], F32) for i in range(2)]
        ps = [st.enter_context(nc.psum_tensor("ps%d" % i, [128, 512], F32)) for i in range(8)]

        S.op("vector", lambda e: e.memset(ones1[:, :], 1.0), writes=["ones1"])
        ci = 0
        di = 0
        for b in range(T // TB):
            tsl = slice(b * TB, (b + 1) * TB)
            S.dma(lambda e, tsl=tsl: e.dma_start(out=xb[:, :, :], in_=xT[:, tsl].rearrange("(k p) t -> p k t", p=128)),
                  writes=["xb"])
            S.dma(lambda e, tsl=tsl: e.dma_start(out=cb1[:, :], in_=comb[:, tsl]), writes=["cb1"])
            S.op("tensor", lambda e: e.matmul(ps[6][:, :], lhsT=ones1[:, :], rhs=cb1[:, :], start=True, stop=True),
                 reads=["ones1", "cb1"], writes=["ps6"])
            S.op("scalar", lambda e: e.activation(out=comb_bc[:, :], in_=ps[6][:, :], func=AF.Copy),
                 reads=["ps6"], writes=["comb_bc"])
            for fi, (fo, fs) in enumerate(fch):
                p = ci % 2
                ci += 1
                S.dma(lambda e, p=p, fo=fo, fs=fs: e.dma_start(out=wgf[p][:, :, 0:fs], in_=wg[:, fo:fo + fs].rearrange("(k p) f -> p k f", p=128)),
                      writes=[("wgf", p)])
                S.dma(lambda e, p=p, fo=fo, fs=fs: e.dma_start(out=wuf[p][:, :, 0:fs], in_=wu[:, fo:fo + fs].rearrange("(k p) f -> p k f", p=128)),
                      writes=[("wuf", p)])
                S.op("gpsimd", lambda e, p=p, fs=fs: e.tensor_copy(out=wgb[p][:, :, 0:fs], in_=wgf[p][:, :, 0:fs]),
                     reads=[("wgf", p)], writes=[("wgb", p)])
                S.op("vector", lambda e, p=p, fs=fs: e.tensor_copy(out=wub[p][:, :, 0:fs], in_=wuf[p][:, :, 0:fs]),
                     reads=[("wuf", p)], writes=[("wub", p)])
                pg, pu = ps[2 * p], ps[2 * p + 1]
                for k in range(KD):
                    S.op("tensor", lambda e, p=p, k=k, fs=fs, pg=pg: e.matmul(pg[0:fs, :], lhsT=wgb[p][:, k, 0:fs], rhs=xb[:, k, :],
                                                                           start=(k == 0), stop=(k == KD - 1)),
                         reads=[("wgb", p), "xb"], writes=[("psg", p)])
                for k in range(KD):
                    S.op("tensor", lambda e, p=p, k=k, fs=fs, pu=pu: e.matmul(pu[0:fs, :], lhsT=wub[p][:, k, 0:fs], rhs=xb[:, k, :],
                                                                           start=(k == 0), stop=(k == KD - 1)),
                         reads=[("wub", p), "xb"], writes=[("psu", p)])
                S.op("scalar", lambda e, p=p, fs=fs, pg=pg: e.activation(out=sg[p][0:fs, :], in_=pg[0:fs, :], func=AF.Silu),
                     reads=[("psg", p)], writes=[("sg", p)])
                S.op("vector", lambda e, p=p, fs=fs, fi=fi, pu=pu: e.tensor_tensor(out=hT[0:fs, fi, :], in0=sg[p][0:fs, :], in1=pu[0:fs, :], op=ALU.mult),
                     reads=[("sg", p), ("psu", p)], writes=[("hT", fi)])
            for n in range(KD):
                p = di % 2
                di += 1
                for fi, (fo, fs) in enumerate(fch):
                    S.dma(lambda e, p=p, fo=fo, fs=fs, fi=fi, n=n: e.dma_start(out=wdf[p][0:fs, fi, :], in_=wd[fo:fo + fs, n * 128:(n + 1) * 128]),
                          writes=[("wdf", p, fi)])
                    S.op("gpsimd", lambda e, p=p, fs=fs, fi=fi: e.tensor_copy(out=wdb[p][0:fs, fi, :], in_=wdf[p][0:fs, fi, :]),
                         reads=[("wdf", p, fi)], writes=[("wdb", p, fi)])
                py = ps[4 + p]
                for fi, (fo, fs) in enumerate(fch):
                    S.op("tensor", lambda e, p=p, fs=fs, fi=fi, py=py: e.matmul(py[:, :], lhsT=wdb[p][0:fs, fi, :], rhs=hT[0:fs, fi, :],
                                                                             start=(fi == 0), stop=(fi == NF - 1)),
                         reads=[("wdb", p, fi), ("hT", fi)], writes=[("psy", p)])
                S.op("vector", lambda e, p=p, py=py: e.tensor_tensor(out=yo[p][:, :], in0=py[:, :], in1=comb_bc[:, :], op=ALU.mult),
                     reads=[("psy", p), "comb_bc"], writes=[("yo", p)])
                S.dma(lambda e, p=p, n=n, tsl=tsl: e.dma_start(out=yT[n * 128:(n + 1) * 128, tsl], in_=yo[p][:, :]),
                      reads=[("yo", p)], writes=[("yT", n, tsl.start)])
        S.emit(nc, st)
    return nc


def build_combine(D, TL, NP):
    from contextlib import ExitStack
    KD = D // 128
    nc = bass.Bass("TRN2", target_bir_lowering=False)
    x1T = nc.dram_tensor("x1T", [D, TL], F32, kind="ExternalInput").ap()
    parts = nc.dram_tensor("parts", [NP * D, TL], F32, kind="ExternalInput").ap()
    gb = nc.dram_tensor("gb", [128, 2 * KD], F32, kind="ExternalInput").ap()
    xo = nc.dram_tensor("xo", [D, TL], F32, kind="ExternalOutput").ap()
    S = Sched()
    with ExitStack() as st:
        sb = lambda name, shape, dt: st.enter_context(nc.sbuf_tensor(name, shape, dt))
        v = sb("v", [128, KD, TL], F32)
        o = sb("o", [128, KD, TL], F32)
        pt = [sb("pt%d" % i, [128, TL], F32) for i in range(2)]
        gbt = sb("gbt", [128, 2 * KD], F32)
        ones_b = sb("ones_b", [128, 128], BF16)
        scr = {"b1": sb("b1", [128, 512], BF16), "b2": sb("b2", [128, 512], BF16),
               "mean": sb("mean", [128, 512], F32), "rstd": sb("rstd", [128, 512], F32), "t": sb("t", [128, 512], F32)}
        ps = [st.enter_context(nc.psum_tensor("ps%d" % i, [128, 512], F32)) for i in range(2)]
        S.op("vector", lambda e: e.memset(ones_b[:, :], 1.0), writes=[("ln", "ones")])
        S.dma(lambda e: e.dma_start(out=gbt[:, :], in_=gb[:, :]), writes=[("ln", "gb")])
        S.dma(lambda e: e.dma_start(out=v[:, :, :], in_=x1T.rearrange("(k p) t -> p k t", p=128)),
              writes=[("ln", "v", k) for k in range(KD)])
        pi = 0
        for k in range(KD):
            S.op("scalar", lambda e, k=k: e.activation(out=v[:, k, :], in_=v[:, k, :], func=AF.Copy, scale=float(ALPHA)),
                 reads=[("ln", "v", k)], writes=[("ln", "v", k)])
            for ei in range(NP):
                p = pi % 2
                pi += 1
                S.dma(lambda e, p=p, ei=ei, k=k: e.dma_start(out=pt[p][:, :], in_=parts[ei * D + k * 128: ei * D + (k + 1) * 128, :]),
                      writes=[("pt", p)])
                S.op("vector", lambda e, p=p, k=k: e.tensor_tensor(out=v[:, k, :], in0=v[:, k, :], in1=pt[p][:, :], op=ALU.add),
                     reads=[("pt", p), ("ln", "v", k)], writes=[("ln", "v", k)])
        _ln_feature_major(S, nc, v, KD, TL, gbt, 0, KD, o, None, scr, ps, ones_b, "ln")
        S.dma(lambda e: e.dma_start(out=xo.rearrange("(k p) t -> p k t", p=128), in_=o[:, :, :]),
              reads=[("ln", "of", k) for k in range(KD)], writes=["xo"])
        S.emit(nc, st)
    return nc


def mix_dims(D, H, E):
    CC = D // 2
    A = H * 128
    off = {"a": 0, "g": CC, "q": 2 * CC, "k": 2 * CC + A, "v": 2 * CC + 2 * A, "f": 2 * CC + 3 * A}
    off["gc"] = off["f"] + H
    off["ga"] = off["gc"] + D
    off["end"] = off["ga"] + D
    return CC, A, off


def build_mix(D, TL, H, E, GS=256):
    from contextlib import ExitStack
    CC, A, off = mix_dims(D, H, E)
    KD, KC = D // 128, CC // 128
    NGO = TL // GS
    NGA = 2 * NGO
    NKT = 2 * TL // 128
    TPG = GS // 128
    INC = off["end"]
    NB = (off["f"] // 128) + 2 * KD
    VO = {"bm": 0, "cb": NB, "cg": NB + KC, "cbeta": NB + 2 * KC, "bco": NB + 3 * KC,
          "l1g": NB + 3 * KC + KD, "l1b": NB + 3 * KC + 2 * KD}
    NV = NB + 3 * KC + 3 * KD
    QS = float(HEAD_DIM) ** -0.5
    BIG = 1.0e30

    nc = bass.Bass("TRN2", target_bir_lowering=False)
    dt_in = lambda n, s, d=F32: nc.dram_tensor(n, s, d, kind="ExternalInput").ap()
    x_own = dt_in("x_own", [D, TL])
    x_pre = dt_in("x_pre", [D, TL])
    w_in = dt_in("w_in", [D, INC])
    w_co = dt_in("w_co", [CC, D])
    w_ao = dt_in("w_ao", [A, D])
    w_o = dt_in("w_o", [D, D])
    vec = dt_in("vec", [128, NV])
    bf = dt_in("bf", [H, 1])
    cw = dt_in("cw", [128, KC * CONV_K])
    w_r = dt_in("w_r", [D, E])
    b_r = dt_in("b_r", [1, E])
    flag = dt_in("flag", [128, 1])
    x1T = nc.dram_tensor("x1T", [D, TL], F32, kind="ExternalOutput").ap()
    x1Tb = nc.dram_tensor("x1Tb", [D, TL], BF16, kind="ExternalOutput").ap()
    comb = nc.dram_tensor("comb", [TL, E], F32, kind="ExternalOutput").ap()

    S = Sched()
    with ExitStack() as st:
        sb = lambda name, shape, dt: st.enter_context(nc.sbuf_tensor(name, shape, dt))
        pst = lambda name, shape, dt: st.enter_context(nc.psum_tensor(name, shape, dt))
        ones_b = sb("ones_b", [128, 128], BF16)
        ones_f = sb("ones_f", [128, 128], F32)
        ident_b = sb("ident_b", [128, 128], BF16)
        ident_f = sb("ident_f", [H, H], F32)
        sel = sb("sel", [H, H, 128], F32)
        dmask = [sb("dmask%d" % i, [128, GS], BF16) for i in range(TPG)]
        vect = sb("vect", [128, NV], F32)
        bqs = sb("bqs", [128, H], F32)
        bft = sb("bft", [H, 1], F32)
        cwt = sb("cwt", [128, KC * CONV_K], F32)
        wrt = sb("wrt", [128, KD, E], F32)
        brt = sb("brt", [1, E], F32)
        flg = sb("flg", [128, 1], F32)
        mb = sb("mb", [128, 1], F32)
        kT = sb("kT", [128, H, 2 * TL], BF16)
        V = sb("V", [128, NKT, A], BF16)
        fT = sb("fT", [H, 2 * TL], F32)
        f2 = sb("f2", [H, 2 * TL], F32)
        cumT = sb("cumT", [H, 2 * TL], F32)
        onesH = sb("onesH", [H, 2 * TL], F32)
        nbias = sb("nbias", [128, NKT, H], F32)
        uhalo = sb("uhalo", [128, KC, 32], F32)
        utail = sb("utail", [128, KC, 32], F32)
        xs = [sb("xs%d" % i, [128, GS], F32) for i in range(2)]
        xb = sb("xb", [128, KD, GS], BF16)
        qT = sb("qT", [128, H, GS], BF16)
        UW = GS + 32
        big = sb("big", [128, max(KD * GS, KC * (UW + GS))], F32)
        u = big[:, 0:KC * UW].rearrange("p (c t) -> p c t", t=UW)
        y = big[:, KC * UW:KC * UW + KC * GS].rearrange("p (c t) -> p c t", t=GS)
        v = big[:, 0:KD * GS].rearrange("p (c t) -> p c t", t=GS)
        z = sb("z", [128, KC, GS], BF16)
        OT = sb("OT", [128, H, GS], BF16)
        mT = sb("mT", [128, KD, GS], BF16)
        vtmp = sb("vtmp", [128, GS], BF16)
        asb = sb("asb", [128, GS], F32)
        sgb = sb("sgb", [128, GS], F32)
        gcb = sb("gcb", [128, GS], F32)
        gab = sb("gab", [128, GS], F32)
        ycb = sb("ycb", [128, GS], F32)
        PT = [sb("PT%d" % i, [128, GS], BF16) for i in range(2)]
        rden = sb("rden", [128, GS], F32)
        stb = [sb("stb%d" % i, [128, GS], BF16) for i in range(2)]
        wf = [sb("wf%d" % i, [128, KD, 128], F32) for i in range(2)]
        wb = [sb("wb%d" % i, [128, KD, 128], BF16) for i in range(2)]
        scr = {"b1": sb("b1", [128, 512], BF16), "b2": sb("b2", [128, 512], BF16),
               "mean": sb("mean", [128, 512], F32), "rstd": sb("rstd", [128, 512], F32), "t": sb("t", [128, 512], F32)}
        rt = {n: sb("rt_" + n, [128, E], F32) for n in ("lg", "eq1", "lg2", "eq2", "c1", "cm")}
        r1 = {n: sb("r1_" + n, [128, 1], F32) for n in ("m1", "m2", "d", "e", "w1", "w2")}
        pp = [pst("pp%d" % i, [128, 512], F32) for i in range(2)]
        psS = [pst("psS%d" % i, [128, 512], F32) for i in range(2)]
        po = pst("po", [128, GS], F32)
        pd = pst("pd", [128, GS], F32)
        pvt = pst("pvt", [128, 128], BF16)

        S.op("vector", lambda e: e.memset(ones_b[:, :], 1.0), writes=["ones_b", ("ln1", "ones"), ("cln", "ones")])
        S.op("vector", lambda e: e.memset(ones_f[:, :], 1.0), writes=["ones_f"])
        S.op("vector", lambda e: e.memset(onesH[:, :], 1.0), writes=["onesH"])
        S.op("gpsimd", lambda e: e.affine_select(out=ident_b[:, :], in_=ones_b[:, :], pattern=[[-1, 128]], compare_op=ALU.is_equal,
                                                 fill=0.0, base=0, channel_multiplier=1),
             reads=["ones_b"], writes=["ident_b"])
        S.op("gpsimd", lambda e: e.affine_select(out=ident_f[:, :], in_=ones_f[0:H, 0:H], pattern=[[-1, H]], compare_op=ALU.is_equal,
                                                 fill=0.0, base=0, channel_multiplier=1),
             reads=["ones_f"], writes=["ident_f"])
        for h in range(H):
            S.op("gpsimd", lambda e, h=h: e.affine_select(out=sel[:, h, :], in_=ones_f[0:H, :], pattern=[[0, 128]], compare_op=ALU.is_equal,
                                                          fill=0.0, base=-h, channel_multiplier=1),
                 reads=["ones_f"], writes=["sel"])
        ones_gs = sb("ones_gs", [128, GS], BF16)
        S.op("vector", lambda e: e.memset(ones_gs[:, :], 1.0), writes=["ones_gs"])
        for i in range(TPG):
            S.op("gpsimd", lambda e, i=i: e.affine_select(out=dmask[i][:, :], in_=ones_gs[:, :], pattern=[[1, GS]],
                                                          compare_op=ALU.is_ge, fill=0.0, base=-128 * i, channel_multiplier=-1),
                 reads=["ones_gs"], writes=[("dmask", i)])
        S.dma(lambda e: e.dma_start(out=vect[:, :], in_=vec[:, :]), writes=["vect", ("ln1", "gb"), ("cln", "gb")])
        S.dma(lambda e: e.dma_start(out=bft[:, :], in_=bf[:, :]), writes=["bft"])
        S.dma(lambda e: e.dma_start(out=cwt[:, :], in_=cw[:, :]), writes=["cwt"])
        S.dma(lambda e: e.dma_start(out=wrt[:, :, :], in_=w_r.rearrange("(k p) e -> p k e", p=128)), writes=["wrt"])
        S.dma(lambda e: e.dma_start(out=brt[:, :], in_=b_r[:, :]), writes=["brt"])
        S.dma(lambda e: e.dma_start(out=flg[:, :], in_=flag[:, :]), writes=["flg"])
        S.op("vector", lambda e: e.tensor_scalar(out=mb[:, :], in0=flg[:, :], scalar1=BIG, scalar2=-BIG, op0=ALU.mult, op1=ALU.add),
             reads=["flg"], writes=["mb"])
        qb0 = VO["bm"] + off["q"] // 128
        S.op("vector", lambda e: e.tensor_scalar(out=bqs[:, :], in0=vect[:, qb0:qb0 + H], scalar1=QS, scalar2=None, op0=ALU.mult),
             reads=["vect"], writes=["bqs"])

        wctr = [0]

        def proj(W, kch, coff, size, act, tsl, n, out_ps, act_key, ps_key):
            p = wctr[0] % 2
            wctr[0] += 1
            S.dma(lambda e: e.dma_start(out=wf[p][:, 0:kch, 0:size], in_=W[:, coff:coff + size].rearrange("(k p) f -> p k f", p=128)),
                  writes=[("wf", p)])
            ceng = "gpsimd" if p == 0 else "vector"
            S.op(ceng, lambda e: e.tensor_copy(out=wb[p][:, 0:kch, 0:size], in_=wf[p][:, 0:kch, 0:size]),
                 reads=[("wf", p)], writes=[("wb", p)])
            for k in range(kch):
                S.op("tensor", lambda e, k=k: e.matmul(out_ps[0:size, 0:n], lhsT=wb[p][:, k, 0:size], rhs=act[:, k, tsl],
                                                      start=(k == 0), stop=(k == kch - 1)),
                     reads=[("wb", p)] + list(act_key), writes=[ps_key])

        def load_xb(src, g):
            for k in range(KD):
                q = k % 2
                S.dma(lambda e, k=k, q=q: e.dma_start(out=xs[q][:, :], in_=src[k * 128:(k + 1) * 128, g * GS:(g + 1) * GS]),
                      writes=[("xs", q)])
                S.op("vector", lambda e, k=k, q=q: e.tensor_copy(out=xb[:, k, :], in_=xs[q][:, :]),
                     reads=[("xs", q)], writes=["xb"])

        ppc = [0]

        def next_pp():
            i = ppc[0] % 2
            ppc[0] += 1
            return pp[i], ("pp", i)

        bcol = lambda name, j: vect[:, VO["bm"] + off[name] // 128 + j: VO["bm"] + off[name] // 128 + j + 1]
        gcol = lambda which, j: vect[:, VO["bm"] + off["f"] // 128 + which * KD + j: VO["bm"] + off["f"] // 128 + which * KD + j + 1]
        full = slice(0, GS)

        for t in range(NGA):
            src, g = (x_pre, t) if t < NGO else (x_own, t - NGO)
            load_xb(src, g)
            tcol = slice(t * GS, (t + 1) * GS)
            for h in range(H):
                ps_, pk = next_pp()
                proj(w_in, KD, off["k"] + h * 128, 128, xb, full, GS, ps_, ["xb"], pk)
                S.op("scalar", lambda e, h=h, ps_=ps_, tcol=tcol: e.activation(out=kT[:, h, tcol], in_=ps_[:, 0:GS], func=AF.Identity,
                                                                            bias=bcol("k", h), scale=1.0),
                     reads=[pk, "vect"], writes=[("kT", h)])
            for h in range(H):
                ps_, pk = next_pp()
                proj(w_in, KD, off["v"] + h * 128, 128, xb, full, GS, ps_, ["xb"], pk)
                S.op("scalar", lambda e, h=h, ps_=ps_: e.activation(out=vtmp[:, :], in_=ps_[:, 0:GS], func=AF.Identity,
                                                                   bias=bcol("v", h), scale=1.0),
                     reads=[pk, "vect"], writes=["vtmp"])
                for j in range(TPG):
                    kt = t * TPG + j
                    S.op("tensor", lambda e, j=j: e.transpose(pvt[:, :], vtmp[:, j * 128:(j + 1) * 128], ident_b[:, :]),
                         reads=["vtmp", "ident_b"], writes=["pvt"])
                    S.op("vector", lambda e, kt=kt, h=h: e.tensor_copy(out=V[:, kt, h * 128:(h + 1) * 128], in_=pvt[:, :]),
                         reads=["pvt"], writes=[("V", kt)])
            ps_, pk = next_pp()
            proj(w_in, KD, off["f"], H, xb, full, GS, ps_, ["xb"], pk)
            S.op("scalar", lambda e, ps_=ps_, tcol=tcol: e.activation(out=fT[:, tcol], in_=ps_[0:H, 0:GS], func=AF.Identity,
                                                                   bias=bft[:, 0:1], scale=1.0),
                 reads=[pk, "bft"], writes=["fT"])
            if t == NGO - 1:
                hs = slice(GS - 32, GS)
                for c in range(KC):
                    ps_, pk = next_pp()
                    proj(w_in, KD, off["a"] + c * 128, 128, xb, hs, 32, ps_, ["xb"], pk)
                    S.op("scalar", lambda e, c=c, ps_=ps_: e.activation(out=asb[:, 0:32], in_=ps_[:, 0:32], func=AF.Identity,
                                                                       bias=bcol("a", c), scale=1.0),
                         reads=[pk, "vect"], writes=["asb"])
                    ps2, pk2 = next_pp()
                    proj(w_in, KD, off["g"] + c * 128, 128, xb, hs, 32, ps2, ["xb"], pk2)
                    S.op("scalar", lambda e, c=c, ps2=ps2: e.activation(out=sgb[:, 0:32], in_=ps2[:, 0:32], func=AF.Sigmoid,
                                                                       bias=bcol("g", c), scale=1.0),
                         reads=[pk2, "vect"], writes=["sgb"])
                    S.op("vector", lambda e, c=c: e.tensor_tensor(out=uhalo[:, c, :], in0=asb[:, 0:32], in1=sgb[:, 0:32], op=ALU.mult),
                         reads=["asb", "sgb"], writes=["uhalo"])
                    S.op("vector", lambda e, c=c: e.tensor_scalar(out=uhalo[:, c, :], in0=uhalo[:, c, :], scalar1=flg[:, 0:1], scalar2=None, op0=ALU.mult),
                         reads=["uhalo", "flg"], writes=["uhalo"])

        S.op("vector", lambda e: e.tensor_scalar(out=f2[:, :], in0=fT[:, :], scalar1=-1.0, scalar2=None, op0=ALU.mult),
             reads=["fT"], writes=["f2"])
        S.op("vector", lambda e: e.tensor_tensor(out=f2[:, :], in0=f2[:, :], in1=fT[:, :], op=ALU.max),
             reads=["fT", "f2"], writes=["f2"])
        S.op("scalar", lambda e: e.activation(out=f2[:, :], in_=f2[:, :], func=AF.Exp, scale=-1.0), reads=["f2"], writes=["f2"])
        S.op("scalar", lambda e: e.activation(out=f2[:, :], in_=f2[:, :], func=AF.Ln, bias=1.0, scale=1.0), reads=["f2"], writes=["f2"])
        S.op("vector", lambda e: e.tensor_scalar(out=fT[:, :], in0=fT[:, :], scalar1=0.0, scalar2=None, op0=ALU.min),
             reads=["fT"], writes=["fT"])
        S.op("vector", lambda e: e.tensor_tensor(out=fT[:, :], in0=fT[:, :], in1=f2[:, :], op=ALU.subtract),
             reads=["fT", "f2"], writes=["fT"])
        S.op("vector", lambda e: e.tensor_tensor_scan(out=cumT[:, :], data0=onesH[:, :], data1=fT[:, :], initial=0.0,
                                                      op0=ALU.mult, op1=ALU.add),
             reads=["fT", "onesH"], writes=["cumT"])
        for kt in range(NKT):
            ps_, pk = next_pp()
            S.op("tensor", lambda e, kt=kt, ps_=ps_: e.transpose(ps_[:, 0:H], cumT[:, kt * 128:(kt + 1) * 128], ident_f[:, :]),
                 reads=["cumT", "ident_f"], writes=[pk])
            if kt < NKT // 2:
                S.op("vector", lambda e, kt=kt, ps_=ps_: e.tensor_scalar(out=nbias[:, kt, :], in0=ps_[:, 0:H], scalar1=-1.0, scalar2=mb[:, 0:1],
                                                                        op0=ALU.mult, op1=ALU.add),
                     reads=[pk, "mb"], writes=["nbias"])
            else:
                S.op("vector", lambda e, kt=kt, ps_=ps_: e.tensor_scalar(out=nbias[:, kt, :], in0=ps_[:, 0:H], scalar1=-1.0, scalar2=None, op0=ALU.mult),
                     reads=[pk], writes=["nbias"])

        ukeys = [("u", c) for c in range(KC)] + [("y", c) for c in range(KC)] + [("cln", "v", c) for c in range(KC)] + [("cln", "of", c) for c in range(KC)]
        vkeys = [("ln1", "v", k) for k in range(KD)] + [("ln1", "of", k) for k in range(KD)]
        sti = [0]
        for gi in range(NGO):
            load_xb(x_own, gi)
            gq = slice(TL + gi * GS, TL + (gi + 1) * GS)
            for c in range(KC):
                ps_, pk = next_pp()
                proj(w_in, KD, off["a"] + c * 128, 128, xb, full, GS, ps_, ["xb"], pk)
                S.op("scalar", lambda e, c=c, ps_=ps_: e.activation(out=asb[:, :], in_=ps_[:, 0:GS], func=AF.Identity, bias=bcol("a", c), scale=1.0),
                     reads=[pk, "vect"], writes=["asb"])
                ps2, pk2 = next_pp()
                proj(w_in, KD, off["g"] + c * 128, 128, xb, full, GS, ps2, ["xb"], pk2)
                S.op("scalar", lambda e, c=c, ps2=ps2: e.activation(out=sgb[:, :], in_=ps2[:, 0:GS], func=AF.Sigmoid, bias=bcol("g", c), scale=1.0),
                     reads=[pk2, "vect"], writes=["sgb"])
                S.op("vector", lambda e, c=c: e.tensor_tensor(out=u[:, c, 32:32 + GS], in0=asb[:, :], in1=sgb[:, :], op=ALU.mult),
                     reads=["asb", "sgb"], writes=[("u", c)] + (vkeys if c == 0 else []))
                hsrc = uhalo if gi == 0 else utail
                S.op("vector", lambda e, c=c, hsrc=hsrc: e.tensor_copy(out=u[:, c, 0:32], in_=hsrc[:, c, :]),
                     reads=["uhalo", "utail"], writes=[("u", c)])
                ceng = "vector"
                for j in range(CONV_K):
                    wcol = cwt[:, c * CONV_K + j: c * CONV_K + j + 1]
                    src = u[:, c, 2 + j: 2 + j + GS]
                    if j == 0:
                        S.op(ceng, lambda e, c=c, wcol=wcol, src=src: e.tensor_scalar(out=y[:, c, :], in0=src, scalar1=wcol,
                                                                                    scalar2=vect[:, VO["cb"] + c: VO["cb"] + c + 1], op0=ALU.mult, op1=ALU.add),
                             reads=[("u", c), "cwt", "vect"], writes=[("y", c), ("cln", "v", c)])
                    else:
                        S.op(ceng, lambda e, c=c, wcol=wcol, src=src: e.scalar_tensor_tensor(out=y[:, c, :], in0=src, scalar=wcol, in1=y[:, c, :],
                                                                                           op0=ALU.mult, op1=ALU.add),
                             reads=[("u", c), "cwt", ("y", c)], writes=[("y", c), ("cln", "v", c)])
                S.op("vector", lambda e, c=c: e.tensor_copy(out=utail[:, c, :], in_=u[:, c, GS:GS + 32]),
                     reads=[("u", c)], writes=["utail"])
            _ln_feature_major(S, nc, y, KC, GS, vect, VO["cg"], VO["cbeta"], y, None, scr, psS, ones_b, "cln", ps_keys=("psS0", "psS1"))
            for c in range(KC):
                S.op("scalar", lambda e, c=c: e.activation(out=z[:, c, :], in_=y[:, c, :], func=AF.Silu),
                     reads=[("cln", "of", c)], writes=[("z", c)])
            for h in range(H):
                ps_, pk = next_pp()
                proj(w_in, KD, off["q"] + h * 128, 128, xb, full, GS, ps_, ["xb"], pk)
                S.op("scalar", lambda e, h=h, ps_=ps_: e.activation(out=qT[:, h, :], in_=ps_[:, 0:GS], func=AF.Identity, bias=bqs[:, h:h + 1], scale=QS),
                     reads=[pk, "bqs"], writes=[("qT", h)])
            nkt_g = NKT // 2 + (gi + 1) * TPG
            for h in range(H):
                for kt in range(nkt_g):
                    sp = kt % 2
                    S.op("tensor", lambda e, h=h, kt=kt, sp=sp: e.matmul(psS[sp][:, 0:GS], lhsT=kT[:, h, kt * 128:(kt + 1) * 128], rhs=qT[:, h, :],
                                                                        start=True, stop=False),
                         reads=[("kT", h), ("qT", h)], writes=["psS%d" % sp])
                    S.op("tensor", lambda e, h=h, sp=sp, gq=gq: e.matmul(psS[sp][:, 0:GS], lhsT=sel[:, h, :], rhs=cumT[:, gq], start=False, stop=True),
                         reads=["sel", "cumT"], writes=["psS%d" % sp])
                    S.op("scalar", lambda e, h=h, kt=kt, sp=sp: e.activation(out=PT[sp][:, :], in_=psS[sp][:, 0:GS], func=AF.Exp,
                                                                            bias=nbias[:, kt, h:h + 1], scale=1.0),
                         reads=["psS%d" % sp, "nbias"], writes=[("PT", sp)])
                    dj = kt - (NKT // 2 + gi * TPG)
                    if dj >= 0:
                        S.op("vector", lambda e, sp=sp, dj=dj: e.tensor_tensor(out=PT[sp][:, :], in0=PT[sp][:, :], in1=dmask[dj][:, :], op=ALU.mult),
                             reads=[("PT", sp), ("dmask", dj)], writes=[("PT", sp)])
                    S.op("tensor", lambda e, h=h, kt=kt, sp=sp, nkt_g=nkt_g: e.matmul(po[:, 0:GS], lhsT=V[:, kt, h * 128:(h + 1) * 128], rhs=PT[sp][:, :],
                                                                        start=(kt == 0), stop=(kt == nkt_g - 1)),
                         reads=[("V", kt), ("PT", sp)], writes=["po"])
                    S.op("tensor", lambda e, kt=kt, sp=sp, nkt_g=nkt_g: e.matmul(pd[:, 0:GS], lhsT=ones_b[:, :], rhs=PT[sp][:, :],
                                                                   start=(kt == 0), stop=(kt == nkt_g - 1)),
                         reads=["ones_b", ("PT", sp)], writes=["pd"])
                S.op("vector", lambda e: e.reciprocal(out=rden[:, :], in_=pd[:, 0:GS]), reads=["pd"], writes=["rden"])
                S.op("vector", lambda e, h=h: e.tensor_tensor(out=OT[:, h, :], in0=po[:, 0:GS], in1=rden[:, :], op=ALU.mult),
                     reads=["po", "rden"], writes=[("OT", h)])
            for n in range(KD):
                ps_, pk = next_pp()
                proj(w_in, KD, off["gc"] + n * 128, 128, xb, full, GS, ps_, ["xb"], pk)
                S.op("scalar", lambda e, n=n, ps_=ps_: e.activation(out=gcb[:, :], in_=ps_[:, 0:GS], func=AF.Sigmoid, bias=gcol(0, n), scale=1.0),
                     reads=[pk, "vect"], writes=["gcb"])
                ps_, pk = next_pp()
                proj(w_in, KD, off["ga"] + n * 128, 128, xb, full, GS, ps_, ["xb"], pk)
                S.op("scalar", lambda e, n=n, ps_=ps_: e.activation(out=gab[:, :], in_=ps_[:, 0:GS], func=AF.Sigmoid, bias=gcol(1, n), scale=1.0),
                     reads=[pk, "vect"], writes=["gab"])
                ps_, pk = next_pp()
                proj(w_co, KC, n * 128, 128, z, full, GS, ps_, [("z", c) for c in range(KC)], pk)
                S.op("scalar", lambda e, n=n, ps_=ps_: e.activation(out=ycb[:, :], in_=ps_[:, 0:GS], func=AF.Identity,
                                                                   bias=vect[:, VO["bco"] + n: VO["bco"] + n + 1], scale=1.0),
                     reads=[pk, "vect"], writes=["ycb"])
                S.op("vector", lambda e: e.tensor_tensor(out=gcb[:, :], in0=gcb[:, :], in1=ycb[:, :], op=ALU.mult),
                     reads=["gcb", "ycb"], writes=["gcb"])
                ps_, pk = next_pp()
                proj(w_ao, H, n * 128, 128, OT, full, GS, ps_, [("OT", h) for h in range(H)], pk)
                S.op("vector", lambda e, ps_=ps_: e.tensor_tensor(out=gab[:, :], in0=gab[:, :], in1=ps_[:, 0:GS], op=ALU.mult),
                     reads=["gab", pk], writes=["gab"])
                S.op("vector", lambda e, n=n: e.tensor_tensor(out=mT[:, n, :], in0=gcb[:, :], in1=gab[:, :], op=ALU.add),
                     reads=["gcb", "gab"], writes=[("mT", n)])
            for n in range(KD):
                ps_, pk = next_pp()
                proj(w_o, KD, n * 128, 128, mT, full, GS, ps_, [("mT", k) for k in range(KD)], pk)
                q = n % 2
                S.dma(lambda e, n=n, q=q, gi=gi: e.dma_start(out=xs[q][:, :], in_=x_own[n * 128:(n + 1) * 128, gi * GS:(gi + 1) * GS]),
                      writes=[("xs", q)])
                S.op("vector", lambda e, n=n, q=q, ps_=ps_: e.scalar_tensor_tensor(out=v[:, n, :], in0=xs[q][:, :], scalar=float(ALPHA), in1=ps_[:, 0:GS],
                                                                                 op0=ALU.mult, op1=ALU.add),
                     reads=[("xs", q), pk], writes=[("ln1", "v", n)] + (ukeys if n == 0 else []))
            _ln_feature_major(S, nc, v, KD, GS, vect, VO["l1g"], VO["l1b"], v, None, scr, psS, ones_b, "ln1", ps_keys=("psS0", "psS1"))
            gsl = slice(gi * GS, (gi + 1) * GS)
            for n in range(KD):
                S.dma(lambda e, n=n, gsl=gsl: e.dma_start(out=x1T[n * 128:(n + 1) * 128, gsl], in_=v[:, n, :]),
                      reads=[("ln1", "of", n)], writes=[("x1T", n, gi)])
                q = sti[0] % 2
                sti[0] += 1
                S.op("scalar", lambda e, n=n, q=q: e.activation(out=stb[q][:, :], in_=v[:, n, :], func=AF.Copy),
                     reads=[("ln1", "of", n)], writes=[("stb", q)])
                S.dma(lambda e, n=n, q=q, gsl=gsl: e.dma_start(out=x1Tb[n * 128:(n + 1) * 128, gsl], in_=stb[q][:, :]),
                      reads=[("stb", q)], writes=[("x1Tb", n, gi)])
            for j in range(TPG):
                ps_, pk = next_pp()
                for k in range(KD):
                    S.op("tensor", lambda e, k=k, j=j, ps_=ps_: e.matmul(ps_[:, 0:E], lhsT=v[:, k, j * 128:(j + 1) * 128], rhs=wrt[:, k, :],
                                                                        start=(k == 0), stop=False),
                         reads=[("ln1", "of", k), "wrt"], writes=[pk])
                S.op("tensor", lambda e, ps_=ps_: e.matmul(ps_[:, 0:E], lhsT=ones_f[0:1, :], rhs=brt[:, :], start=False, stop=True),
                     reads=["ones_f", "brt"], writes=[pk])
                R, r = rt, r1
                S.op("vector", lambda e, ps_=ps_: e.tensor_copy(out=R["lg"][:, :], in_=ps_[:, 0:E]), reads=[pk], writes=["r_lg"])
                S.op("vector", lambda e: e.reduce_max(out=r["m1"][:, :], in_=R["lg"][:, :], axis=mybir.AxisListType.X), reads=["r_lg"], writes=["r_m1"])
                S.op("vector", lambda e: e.tensor_scalar(out=R["eq1"][:, :], in0=R["lg"][:, :], scalar1=r["m1"][:, 0:1], scalar2=None, op0=ALU.is_equal),
                     reads=["r_lg", "r_m1"], writes=["r_eq1"])
                S.op("vector", lambda e: e.scalar_tensor_tensor(out=R["lg2"][:, :], in0=R["eq1"][:, :], scalar=-BIG, in1=R["lg"][:, :], op0=ALU.mult, op1=ALU.add),
                     reads=["r_eq1", "r_lg"], writes=["r_lg2"])
                S.op("vector", lambda e: e.reduce_max(out=r["m2"][:, :], in_=R["lg2"][:, :], axis=mybir.AxisListType.X), reads=["r_lg2"], writes=["r_m2"])
                S.op("vector", lambda e: e.tensor_scalar(out=R["eq2"][:, :], in0=R["lg2"][:, :], scalar1=r["m2"][:, 0:1], scalar2=None, op0=ALU.is_equal),
                     reads=["r_lg2", "r_m2"], writes=["r_eq2"])
                S.op("vector", lambda e: e.tensor_tensor(out=r["d"][:, :], in0=r["m2"][:, :], in1=r["m1"][:, :], op=ALU.subtract),
                     reads=["r_m1", "r_m2"], writes=["r_d"])
                S.op("scalar", lambda e: e.activation(out=r["e"][:, :], in_=r["d"][:, :], func=AF.Exp), reads=["r_d"], writes=["r_e"])
                S.op("vector", lambda e: e.tensor_scalar(out=r["w1"][:, :], in0=r["e"][:, :], scalar1=1.0, scalar2=None, op0=ALU.add),
                     reads=["r_e"], writes=["r_w1"])
                S.op("vector", lambda e: e.reciprocal(out=r["w1"][:, :], in_=r["w1"][:, :]), reads=["r_w1"], writes=["r_w1"])
                S.op("vector", lambda e: e.tensor_tensor(out=r["w2"][:, :], in0=r["e"][:, :], in1=r["w1"][:, :], op=ALU.mult),
                     reads=["r_e", "r_w1"], writes=["r_w2"])
                S.op("vector", lambda e: e.tensor_scalar(out=R["c1"][:, :], in0=R["eq1"][:, :], scalar1=r["w1"][:, 0:1], scalar2=None, op0=ALU.mult),
                     reads=["r_eq1", "r_w1"], writes=["r_c1"])
                S.op("vector", lambda e: e.scalar_tensor_tensor(out=R["cm"][:, :], in0=R["eq2"][:, :], scalar=r["w2"][:, 0:1], in1=R["c1"][:, :],
                                                                op0=ALU.mult, op1=ALU.add),
                     reads=["r_eq2", "r_w2", "r_c1"], writes=["r_cm"])
                t0 = gi * GS + j * 128
                S.dma(lambda e, t0=t0: e.dma_start(out=comb[t0:t0 + 128, :], in_=R["cm"][:, :]), reads=["r_cm"], writes=[("comb", t0)])
        S.emit(nc, st)
    return nc


_PROGS = {}


def _prog(key, fn):
    if key not in _PROGS:
        _PROGS[key] = fn()
    return _PROGS[key]


def _launch(nc, in_maps):
    res = run_bass_kernel_spmd(nc, in_maps, core_ids=list(range(NCORES)))
    return res.results


def _pk(v):
    return np.ascontiguousarray(np.asarray(v, np.float32).reshape(-1, 128).T)


def kernel(x, w_in, b_in, conv_w, conv_b, conv_ln_g, conv_ln_b, w_conv_out, b_conv_out,
           w_attn_out, w_o, ln1_g, ln1_b, ffn_wg, ffn_wu, ffn_wd, router_w, router_b,
           exp_wg, exp_wu, exp_wd, ln2_g, ln2_b):
    D, H, E, TL = D_MODEL, N_HEADS, N_EXPERTS, SEQ // 2
    T = BATCH * SEQ
    CC, A, off = mix_dims(D, H, E)
    KC = CC // 128
    x = np.asarray(x, np.float32)
    mix = _prog("mix", lambda: build_mix(D, TL, H, E))
    cmb = _prog("comb", lambda: build_combine(D, TL, NCORES))
    FS = D_FF // NCORES
    ffn_d = _prog("ffn_d", lambda: build_ffn(D, T, FS))
    ffn_e = _prog("ffn_e", lambda: build_ffn(D, T, D_FF))
    xcur = [np.ascontiguousarray(x[c // 2, (c % 2) * TL:(c % 2 + 1) * TL, :].T) for c in range(NCORES)]
    zeros_pre = np.zeros((D, TL), np.float32)
    ones_comb = np.ones((1, T), np.float32)
    for l in range(DEPTH):
        j = l // 2
        b = np.asarray(b_in[l], np.float32)
        bm = np.concatenate([b[:off["f"]], b[off["gc"]:]])
        vec = np.concatenate([_pk(bm), _pk(conv_b[l]), _pk(conv_ln_g[l]), _pk(conv_ln_b[l]), _pk(b_conv_out[l]),
                              _pk(ln1_g[l]), _pk(ln1_b[l])], axis=1)
        cw = np.ascontiguousarray(np.asarray(conv_w[l], np.float32).T.reshape(KC, 128, CONV_K).transpose(1, 0, 2).reshape(128, KC * CONV_K))
        bfv = np.ascontiguousarray(b[off["f"]:off["gc"]].reshape(H, 1))
        maps = []
        for c in range(NCORES):
            odd = c % 2 == 1
            maps.append({"x_own": xcur[c], "x_pre": xcur[c - 1] if odd else zeros_pre,
                         "w_in": np.asarray(w_in[l], np.float32), "w_co": np.asarray(w_conv_out[l], np.float32),
                         "w_ao": np.asarray(w_attn_out[l], np.float32), "w_o": np.asarray(w_o[l], np.float32),
                         "vec": vec, "bf": bfv, "cw": cw, "w_r": np.asarray(router_w[j], np.float32),
                         "b_r": np.asarray(router_b[j], np.float32).reshape(1, E),
                         "flag": np.full((128, 1), 1.0 if odd else 0.0, np.float32)})
        r = _launch(mix, maps)
        x1T = [np.asarray(r[c]["x1T"]) for c in range(NCORES)]
        xall = np.ascontiguousarray(np.concatenate([np.asarray(r[c]["x1Tb"]) for c in range(NCORES)], axis=1))
        maps = []
        if l % 2 == 0:
            for e in range(NCORES):
                maps.append({"xT": xall, "comb": ones_comb,
                             "wg": np.ascontiguousarray(ffn_wg[j][:, e * FS:(e + 1) * FS]),
                             "wu": np.ascontiguousarray(ffn_wu[j][:, e * FS:(e + 1) * FS]),
                             "wd": np.ascontiguousarray(ffn_wd[j][e * FS:(e + 1) * FS, :])})
            r = _launch(ffn_d, maps)
        else:
            call = np.concatenate([np.asarray(r[c]["comb"]) for c in range(NCORES)], axis=0)
            for e in range(NCORES):
                maps.append({"xT": xall, "comb": np.ascontiguousarray(call[:, e].reshape(1, T)),
                             "wg": np.asarray(exp_wg[j][e], np.float32), "wu": np.asarray(exp_wu[j][e], np.float32),
                             "wd": np.asarray(exp_wd[j][e], np.float32)})
            r = _launch(ffn_e, maps)
        yT = [np.asarray(r[e]["yT"]) for e in range(NCORES)]
        gb = np.concatenate([_pk(ln2_g[l]), _pk(ln2_b[l])], axis=1)
        maps = []
        for c in range(NCORES):
            parts = np.ascontiguousarray(np.concatenate([yT[e][:, c * TL:(c + 1) * TL] for e in range(NCORES)], axis=0))
            maps.append({"x1T": x1T[c], "parts": parts, "gb": gb})
        r = _launch(cmb, maps)
        xcur = [np.asarray(r[c]["xo"]) for c in range(NCORES)]
    out = np.empty((BATCH, SEQ, D), np.float32)
    for c in range(NCORES):
        out[c // 2, (c % 2) * TL:(c % 2 + 1) * TL, :] = xcur[c].T
    return out
```

```python
import numpy as np
import ml_dtypes
import concourse.bass as bass
import concourse.mybir as mybir
from concourse.bass_utils import run_bass_kernel_spmd

F32 = mybir.dt.float32
BF16 = mybir.dt.bfloat16
AF = mybir.ActivationFunctionType
ALU = mybir.AluOpType

D_MODEL = 2048
BATCH = 4
SEQ = 2048
DEPTH = 4
N_HEADS = 8
HEAD_DIM = 128
CONV_K = 31
D_FF = 5632
N_EXPERTS = 8
LN_EPS = 1e-5
ALPHA = (2 * DEPTH) ** 0.25
NCORES = 8


class Sched:
    ENGS = ("tensor", "scalar", "vector", "gpsimd", "sync")
    NSTREAM = 8
    NGSTREAM = 6

    def __init__(self):
        self.ops = {e: [] for e in self.ENGS}
        self.cnt = {}
        self.last_w = {}
        self.readers = {}
        self.waited = {e: {} for e in self.ENGS}
        self.stream_rr = 0
        self.gstream_rr = 0
        self.out_dmas = []

    def _deps(self, reads, writes):
        deps = []
        for b in reads:
            if b in self.last_w:
                deps.append(self.last_w[b])
        for b in writes:
            if b in self.last_w:
                deps.append(self.last_w[b])
            deps.extend(self.readers.get(b, ()))
        return deps

    def _commit(self, tok, reads, writes):
        for b in reads:
            self.readers.setdefault(b, []).append(tok)
        for b in writes:
            self.last_w[b] = tok
            self.readers[b] = []

    def _filter(self, eng, deps):
        best = {}
        for s, v in deps:
            if v > best.get(s, 0):
                best[s] = v
        out = []
        w = self.waited[eng]
        for s, v in best.items():
            if w.get(s, 0) < v:
                w[s] = v
                out.append((s, v))
        return out

    def op(self, eng, fn, reads=(), writes=()):
        sem = "c_" + eng
        deps = self._deps(reads, writes)
        val = self.cnt.get(sem, 0) + 1
        self.cnt[sem] = val
        self.ops[eng].append((self._filter(eng, deps), fn, sem, 1))
        self._commit((sem, val), reads, writes)

    def dma(self, fn, reads=(), writes=(), eng="sync", is_out=False):
        if eng == "gpsimd":
            st = self.gstream_rr
            self.gstream_rr = (st + 1) % self.NGSTREAM
            sem = "g_%d" % st
        else:
            st = self.stream_rr
            self.stream_rr = (st + 1) % self.NSTREAM
            sem = "d_%d" % st
        deps = self._deps(reads, writes)
        prev = self.cnt.get(sem, 0)
        if prev:
            deps.append((sem, prev))
        val = prev + 16
        self.cnt[sem] = val
        self.ops[eng].append((self._filter(eng, deps), fn, sem, 16))
        self._commit((sem, val), reads, writes)

    def emit(self, nc, stack):
        names = ["c_" + e for e in self.ENGS if e != "sync"] + ["d_%d" % i for i in range(self.NSTREAM)] + ["g_%d" % i for i in range(self.NGSTREAM)]
        sems = {n: stack.enter_context(nc.semaphore(n)) for n in names}
        finals = [(s, v) for s, v in self.cnt.items() if v > 0]
        block = stack.enter_context(nc.Block())
        ops = self.ops

        def run(eng_obj, lst, final=False):
            for waits, fn, sem, inc in lst:
                for s, v in waits:
                    eng_obj.wait_ge(sems[s], v)
                ins = fn(eng_obj)
                ins.then_inc(sems[sem], inc)
            if final:
                for s, v in finals:
                    eng_obj.wait_ge(sems[s], v)

        @block.tensor
        def _(e):
            run(e, ops["tensor"])

        @block.scalar
        def _(e):
            run(e, ops["scalar"])

        @block.vector
        def _(e):
            run(e, ops["vector"])

        @block.gpsimd
        def _(e):
            run(e, ops["gpsimd"])

        @block.sync
        def _(e):
            run(e, ops["sync"], final=True)


def _chunks(n):
    out = []
    o = 0
    while o < n:
        s = min(128, n - o)
        out.append((o, s))
        o += s
    return out


def _ln_feature_major(S, nc, v, nk, ntok, gbt, goff, boff, out_f32, out_bf, scr, ps, ones_b, tag, ps_keys=None):
    pk0, pk1 = ps_keys if ps_keys else ((tag, 'ps0'), (tag, 'ps1'))
    nfeat = float(nk * 128)
    for g0 in range(0, ntok, 512):
        n = min(512, ntok - g0)
        sl = slice(g0, g0 + n)
        for k in range(nk):
            S.op("vector", lambda e, k=k, n=n, sl=sl: e.tensor_copy(out=scr["b1"][:, 0:n], in_=v[:, k, sl]),
                 reads=[(tag, "v", k)], writes=[(tag, "b1")])
            S.op("scalar", lambda e, k=k, n=n, sl=sl: e.activation(out=scr["b2"][:, 0:n], in_=v[:, k, sl], func=AF.Square),
                 reads=[(tag, "v", k)], writes=[(tag, "b2")])
            S.op("tensor", lambda e, k=k, n=n, sl=sl: e.matmul(ps[0][:, 0:n], lhsT=ones_b[:, :], rhs=scr["b1"][:, 0:n],
                                                  start=(k == 0), stop=(k == nk - 1)),
                 reads=[(tag, "b1"), (tag, "ones")], writes=[pk0])
            S.op("tensor", lambda e, k=k, n=n, sl=sl: e.matmul(ps[1][:, 0:n], lhsT=ones_b[:, :], rhs=scr["b2"][:, 0:n],
                                                  start=(k == 0), stop=(k == nk - 1)),
                 reads=[(tag, "b2"), (tag, "ones")], writes=[pk1])
        S.op("scalar", lambda e, n=n, sl=sl: e.activation(out=scr["mean"][:, 0:n], in_=ps[0][:, 0:n], func=AF.Copy, scale=1.0 / nfeat),
             reads=[pk0], writes=[(tag, "mean")])
        S.op("vector", lambda e, n=n, sl=sl: e.tensor_tensor(out=scr["t"][:, 0:n], in0=scr["mean"][:, 0:n], in1=scr["mean"][:, 0:n], op=ALU.mult),
             reads=[(tag, "mean")], writes=[(tag, "t")])
        S.op("vector", lambda e, n=n, sl=sl: e.scalar_tensor_tensor(out=scr["rstd"][:, 0:n], in0=ps[1][:, 0:n], scalar=1.0 / nfeat,
                                                        in1=scr["t"][:, 0:n], op0=ALU.mult, op1=ALU.subtract),
             reads=[pk1, (tag, "t")], writes=[(tag, "rstd")])
        S.op("vector", lambda e, n=n, sl=sl: e.tensor_scalar(out=scr["rstd"][:, 0:n], in0=scr["rstd"][:, 0:n], scalar1=0.0, scalar2=LN_EPS,
                                                 op0=ALU.max, op1=ALU.add),
             reads=[(tag, "rstd")], writes=[(tag, "rstd")])
        S.op("scalar", lambda e, n=n, sl=sl: e.activation(out=scr["rstd"][:, 0:n], in_=scr["rstd"][:, 0:n], func=AF.Sqrt),
             reads=[(tag, "rstd")], writes=[(tag, "rstd")])
        S.op("vector", lambda e, n=n, sl=sl: e.reciprocal(out=scr["rstd"][:, 0:n], in_=scr["rstd"][:, 0:n]),
             reads=[(tag, "rstd")], writes=[(tag, "rstd")])
        for k in range(nk):
            S.op("vector", lambda e, k=k, n=n, sl=sl: e.tensor_tensor(out=scr["t"][:, 0:n], in0=v[:, k, sl], in1=scr["mean"][:, 0:n], op=ALU.subtract),
                 reads=[(tag, "v", k), (tag, "mean")], writes=[(tag, "t")])
            S.op("vector", lambda e, k=k, n=n, sl=sl: e.tensor_tensor(out=scr["t"][:, 0:n], in0=scr["t"][:, 0:n], in1=scr["rstd"][:, 0:n], op=ALU.mult),
                 reads=[(tag, "t"), (tag, "rstd")], writes=[(tag, "t")])
            if out_f32 is not None:
                S.op("scalar", lambda e, k=k, n=n, sl=sl: e.activation(out=out_f32[:, k, sl], in_=scr["t"][:, 0:n], func=AF.Identity,
                                                          scale=gbt[:, goff + k:goff + k + 1], bias=gbt[:, boff + k:boff + k + 1]),
                     reads=[(tag, "t"), (tag, "gb")], writes=[(tag, "of", k)])
            if out_bf is not None:
                S.op("scalar", lambda e, k=k, n=n, sl=sl: e.activation(out=out_bf[:, k, sl], in_=scr["t"][:, 0:n], func=AF.Identity,
                                                          scale=gbt[:, goff + k:goff + k + 1], bias=gbt[:, boff + k:boff + k + 1]),
                     reads=[(tag, "t"), (tag, "gb")], writes=[(tag, "ob", k)])


def ffn_weight_layout(wg, wu, wd):
    D, FL = wg.shape
    KD = D // 128
    NF = (FL + 127) // 128
    FP = NF * 128

    def up(w):
        wp = np.zeros((D, FP), np.float32)
        wp[:, :FL] = w
        return np.ascontiguousarray(wp.reshape(KD, 128, NF, 128).transpose(2, 1, 0, 3))

    dp = np.zeros((FP, D), np.float32)
    dp[:FL] = wd
    wdp = np.ascontiguousarray(dp.reshape(NF, 128, KD, 128).transpose(2, 1, 0, 3))
    return up(wg), up(wu), wdp


def build_ffn(D, T, FL, TB=1024):
    from contextlib import ExitStack
    KD = D // 128
    fch = _chunks(FL)
    NF = len(fch)
    NG = TB // 512
    nc = bass.Bass("TRN2", target_bir_lowering=False)
    xT = nc.dram_tensor("xT", [D, T], BF16, kind="ExternalInput").ap()
    comb = nc.dram_tensor("comb", [1, T], F32, kind="ExternalInput").ap()
    wg = nc.dram_tensor("wg", [NF, 128, KD, 128], F32, kind="ExternalInput").ap()
    wu = nc.dram_tensor("wu", [NF, 128, KD, 128], F32, kind="ExternalInput").ap()
    wd = nc.dram_tensor("wd", [KD, 128, NF, 128], F32, kind="ExternalInput").ap()
    yT = nc.dram_tensor("yT", [D, T], F32, kind="ExternalOutput").ap()
    S = Sched()
    with ExitStack() as st:
        sb = lambda name, shape, dt: st.enter_context(nc.sbuf_tensor(name, shape, dt))
        xb = sb("xb", [128, KD, TB], BF16)
        hT = sb("hT", [128, NF, TB], BF16)
        cb1 = sb("cb1", [1, TB], F32)
        ones1 = sb("ones1", [1, 128], F32)
        comb_bc = sb("comb_bc", [128, TB], F32)
        NWB = 3
        wgb = [sb("wgb%d" % i, [128, KD, 128], BF16) for i in range(NWB)]
        wub = [sb("wub%d" % i, [128, KD, 128], BF16) for i in range(NWB)]
        wdb = [sb("wdb%d" % i, [128, NF, 128], BF16) for i in range(2)]
        sg = [sb("sg%d" % i, [128, 512], F32) for i in range(2)]
        yo = [sb("yo%d" % i, [128, 512], F32) for i in range(2)]
        ps = [st.enter_context(nc.psum_tensor("ps%d" % i, [128, 512], F32)) for i in range(8)]

        S.op("vector", lambda e: e.memset(ones1[:, :], 1.0), writes=["ones1"])
        ci = 0
        di = 0
        ei = 0
        for b in range(T // TB):
            t0 = b * TB
            S.dma(lambda e, t0=t0: e.dma_start(out=xb[:, :, :], in_=xT[:, t0:t0 + TB].rearrange("(k p) t -> p k t", p=128)),
                  writes=["xb"])
            S.dma(lambda e, t0=t0: e.dma_start(out=cb1[:, :], in_=comb[:, t0:t0 + TB]), writes=["cb1"])
            for g in range(NG):
                gs = slice(g * 512, (g + 1) * 512)
                S.op("tensor", lambda e, gs=gs: e.matmul(ps[0][:, :], lhsT=ones1[:, :], rhs=cb1[:, gs], start=True, stop=True),
                     reads=["ones1", "cb1"], writes=[("ps", 0)])
                S.op("scalar", lambda e, gs=gs: e.activation(out=comb_bc[:, gs], in_=ps[0][:, :], func=AF.Copy),
                     reads=[("ps", 0)], writes=["comb_bc"])
            for fi, (fo, fs) in enumerate(fch):
                p = ci % NWB
                q = ci % 2
                ci += 1
                S.dma(lambda e, p=p, fi=fi: e.dma_start(out=wgb[p][:, :, :], in_=wg[fi, :, :, :]), writes=[("wgb", p)], eng="gpsimd")
                S.dma(lambda e, p=p, fi=fi: e.dma_start(out=wub[p][:, :, :], in_=wu[fi, :, :, :]), writes=[("wub", p)], eng="gpsimd")
                for g in range(NG):
                    gs = slice(g * 512, (g + 1) * 512)
                    bg, bu = 4 * q + 2 * g, 4 * q + 2 * g + 1

                    def mm_g(e, p=p, fs=fs, gs=gs, bg=bg):
                        for k in range(KD):
                            ins = e.matmul(ps[bg][0:fs, :], lhsT=wgb[p][:, k, 0:fs], rhs=xb[:, k, gs], start=(k == 0), stop=(k == KD - 1))
                        return ins

                    def mm_u(e, p=p, fs=fs, gs=gs, bu=bu):
                        for k in range(KD):
                            ins = e.matmul(ps[bu][0:fs, :], lhsT=wub[p][:, k, 0:fs], rhs=xb[:, k, gs], start=(k == 0), stop=(k == KD - 1))
                        return ins

                    S.op("tensor", mm_g, reads=[("wgb", p), "xb"], writes=[("ps", bg)])
                    S.op("tensor", mm_u, reads=[("wub", p), "xb"], writes=[("ps", bu)])
                    r = ei % 2
                    ei += 1
                    S.op("scalar", lambda e, r=r, fs=fs, bg=bg: e.activation(out=sg[r][0:fs, :], in_=ps[bg][0:fs, :], func=AF.Silu),
                         reads=[("ps", bg)], writes=[("sg", r)])
                    S.op("vector", lambda e, r=r, fs=fs, fi=fi, gs=gs, bu=bu: e.tensor_tensor(out=hT[0:fs, fi, gs], in0=sg[r][0:fs, :], in1=ps[bu][0:fs, :], op=ALU.mult),
                         reads=[("sg", r), ("ps", bu)], writes=[("hT", fi, g)])
            for n in range(KD):
                p = di % 2
                S.dma(lambda e, p=p, n=n: e.dma_start(out=wdb[p][:, :, :], in_=wd[n, :, :, :]), writes=[("wdb", p)], eng="gpsimd")
                for g in range(NG):
                    gs = slice(g * 512, (g + 1) * 512)
                    bk = (di * NG + g) % 8

                    def mm_d(e, p=p, gs=gs, bk=bk):
                        for fi, (fo, fs) in enumerate(fch):
                            ins = e.matmul(ps[bk][:, :], lhsT=wdb[p][0:fs, fi, :], rhs=hT[0:fs, fi, gs], start=(fi == 0), stop=(fi == NF - 1))
                        return ins

                    S.op("tensor", mm_d, reads=[("wdb", p)] + [("hT", fi, g) for fi in range(NF)], writes=[("ps", bk)])
                    r = ei % 2
                    ei += 1
                    S.op("vector", lambda e, r=r, gs=gs, bk=bk: e.tensor_tensor(out=yo[r][:, :], in0=ps[bk][:, :], in1=comb_bc[:, gs], op=ALU.mult),
                         reads=[("ps", bk), "comb_bc"], writes=[("yo", r)])
                    S.dma(lambda e, r=r, n=n, t0=t0, g=g: e.dma_start(out=yT[n * 128:(n + 1) * 128, t0 + g * 512:t0 + (g + 1) * 512], in_=yo[r][:, :]),
                          reads=[("yo", r)], writes=[("yT", n, t0, g)])
                di += 1
        S.emit(nc, st)
    return nc


def build_combine(D, TL, NP):
    from contextlib import ExitStack
    KD = D // 128
    nc = bass.Bass("TRN2", target_bir_lowering=False)
    x1T = nc.dram_tensor("x1T", [D, TL], F32, kind="ExternalInput").ap()
    parts = nc.dram_tensor("parts", [NP * D, TL], F32, kind="ExternalInput").ap()
    gb = nc.dram_tensor("gb", [128, 2 * KD], F32, kind="ExternalInput").ap()
    xo = nc.dram_tensor("xo", [D, TL], F32, kind="ExternalOutput").ap()
    S = Sched()
    with ExitStack() as st:
        sb = lambda name, shape, dt: st.enter_context(nc.sbuf_tensor(name, shape, dt))
        v = sb("v", [128, KD, TL], F32)
        o = sb("o", [128, KD, TL], F32)
        pt = [sb("pt%d" % i, [128, TL], F32) for i in range(2)]
        gbt = sb("gbt", [128, 2 * KD], F32)
        ones_b = sb("ones_b", [128, 128], BF16)
        scr = {"b1": sb("b1", [128, 512], BF16), "b2": sb("b2", [128, 512], BF16),
               "mean": sb("mean", [128, 512], F32), "rstd": sb("rstd", [128, 512], F32), "t": sb("t", [128, 512], F32)}
        ps = [st.enter_context(nc.psum_tensor("ps%d" % i, [128, 512], F32)) for i in range(2)]
        S.op("vector", lambda e: e.memset(ones_b[:, :], 1.0), writes=[("ln", "ones")])
        S.dma(lambda e: e.dma_start(out=gbt[:, :], in_=gb[:, :]), writes=[("ln", "gb")])
        S.dma(lambda e: e.dma_start(out=v[:, :, :], in_=x1T.rearrange("(k p) t -> p k t", p=128)),
              writes=[("ln", "v", k) for k in range(KD)])
        pi = 0
        for k in range(KD):
            S.op("scalar", lambda e, k=k: e.activation(out=v[:, k, :], in_=v[:, k, :], func=AF.Copy, scale=float(ALPHA)),
                 reads=[("ln", "v", k)], writes=[("ln", "v", k)])
            for ei in range(NP):
                p = pi % 2
                pi += 1
                S.dma(lambda e, p=p, ei=ei, k=k: e.dma_start(out=pt[p][:, :], in_=parts[ei * D + k * 128: ei * D + (k + 1) * 128, :]),
                      writes=[("pt", p)])
                S.op("vector", lambda e, p=p, k=k: e.tensor_tensor(out=v[:, k, :], in0=v[:, k, :], in1=pt[p][:, :], op=ALU.add),
                     reads=[("pt", p), ("ln", "v", k)], writes=[("ln", "v", k)])
        _ln_feature_major(S, nc, v, KD, TL, gbt, 0, KD, o, None, scr, ps, ones_b, "ln")
        S.dma(lambda e: e.dma_start(out=xo.rearrange("(k p) t -> p k t", p=128), in_=o[:, :, :]),
              reads=[("ln", "of", k) for k in range(KD)], writes=["xo"])
        S.emit(nc, st)
    return nc


def mix_dims(D, H, E):
    CC = D // 2
    A = H * 128
    off = {"a": 0, "g": CC, "q": 2 * CC, "k": 2 * CC + A, "v": 2 * CC + 2 * A, "f": 2 * CC + 3 * A}
    off["gc"] = off["f"] + H
    off["ga"] = off["gc"] + D
    off["end"] = off["ga"] + D
    return CC, A, off


def build_mix(D, TL, H, E, GS=256):
    from contextlib import ExitStack
    CC, A, off = mix_dims(D, H, E)
    KD, KC = D // 128, CC // 128
    NGO = TL // GS
    NGA = 2 * NGO
    NKT = 2 * TL // 128
    TPG = GS // 128
    INC = off["end"]
    NB = (off["f"] // 128) + 2 * KD
    VO = {"bm": 0, "cb": NB, "cg": NB + KC, "cbeta": NB + 2 * KC, "bco": NB + 3 * KC,
          "l1g": NB + 3 * KC + KD, "l1b": NB + 3 * KC + 2 * KD}
    NV = NB + 3 * KC + 3 * KD
    QS = float(HEAD_DIM) ** -0.5
    BIG = 1.0e30

    nc = bass.Bass("TRN2", target_bir_lowering=False)
    dt_in = lambda n, s, d=F32: nc.dram_tensor(n, s, d, kind="ExternalInput").ap()
    x_own = dt_in("x_own", [D, TL])
    x_pre = dt_in("x_pre", [D, TL])
    w_in = dt_in("w_in", [D, INC])
    w_co = dt_in("w_co", [CC, D])
    w_ao = dt_in("w_ao", [A, D])
    w_o = dt_in("w_o", [D, D])
    vec = dt_in("vec", [128, NV])
    bf = dt_in("bf", [H, 1])
    cw = dt_in("cw", [128, KC * CONV_K])
    w_r = dt_in("w_r", [D, E])
    b_r = dt_in("b_r", [1, E])
    flag = dt_in("flag", [128, 1])
    x1T = nc.dram_tensor("x1T", [D, TL], F32, kind="ExternalOutput").ap()
    x1Tb = nc.dram_tensor("x1Tb", [D, TL], BF16, kind="ExternalOutput").ap()
    comb = nc.dram_tensor("comb", [TL, E], F32, kind="ExternalOutput").ap()

    S = Sched()
    with ExitStack() as st:
        sb = lambda name, shape, dt: st.enter_context(nc.sbuf_tensor(name, shape, dt))
        pst = lambda name, shape, dt: st.enter_context(nc.psum_tensor(name, shape, dt))
        ones_b = sb("ones_b", [128, 128], BF16)
        ones_f = sb("ones_f", [128, 128], F32)
        ident_b = sb("ident_b", [128, 128], BF16)
        ident_f = sb("ident_f", [H, H], F32)
        sel = sb("sel", [H, H, 128], F32)
        dmask = [sb("dmask%d" % i, [128, GS], BF16) for i in range(TPG)]
        vect = sb("vect", [128, NV], F32)
        bqs = sb("bqs", [128, H], F32)
        bft = sb("bft", [H, 1], F32)
        cwt = sb("cwt", [128, KC * CONV_K], F32)
        wrt = sb("wrt", [128, KD, E], F32)
        brt = sb("brt", [1, E], F32)
        flg = sb("flg", [128, 1], F32)
        mb = sb("mb", [128, 1], F32)
        kT = sb("kT", [128, H, 2 * TL], BF16)
        V = sb("V", [128, NKT, A], BF16)
        fT = sb("fT", [H, 2 * TL], F32)
        f2 = sb("f2", [H, 2 * TL], F32)
        cumT = sb("cumT", [H, 2 * TL], F32)
        onesH = sb("onesH", [H, 2 * TL], F32)
        nbias = sb("nbias", [128, NKT, H], F32)
        uhalo = sb("uhalo", [128, KC, 32], F32)
        utail = sb("utail", [128, KC, 32], F32)
        xs = [sb("xs%d" % i, [128, GS], F32) for i in range(2)]
        xb = sb("xb", [128, KD, GS], BF16)
        qT = sb("qT", [128, H, GS], BF16)
        UW = GS + 32
        big = sb("big", [128, max(KD * GS, KC * (UW + GS))], F32)
        u = big[:, 0:KC * UW].rearrange("p (c t) -> p c t", t=UW)
        y = big[:, KC * UW:KC * UW + KC * GS].rearrange("p (c t) -> p c t", t=GS)
        v = big[:, 0:KD * GS].rearrange("p (c t) -> p c t", t=GS)
        z = sb("z", [128, KC, GS], BF16)
        OT = sb("OT", [128, H, GS], BF16)
        mT = sb("mT", [128, KD, GS], BF16)
        vtmp = sb("vtmp", [128, GS], BF16)
        asb = sb("asb", [128, GS], F32)
        sgb = sb("sgb", [128, GS], F32)
        gcb = sb("gcb", [128, GS], F32)
        gab = sb("gab", [128, GS], F32)
        ycb = sb("ycb", [128, GS], F32)
        PT = [sb("PT%d" % i, [128, GS], BF16) for i in range(2)]
        rden = sb("rden", [128, GS], F32)
        stb = [sb("stb%d" % i, [128, GS], BF16) for i in range(2)]
        wf = [sb("wf%d" % i, [128, KD, 128], F32) for i in range(2)]
        wb = [sb("wb%d" % i, [128, KD, 128], BF16) for i in range(2)]
        scr = {"b1": sb("b1", [128, 512], BF16), "b2": sb("b2", [128, 512], BF16),
               "mean": sb("mean", [128, 512], F32), "rstd": sb("rstd", [128, 512], F32), "t": sb("t", [128, 512], F32)}
        rt = {n: sb("rt_" + n, [128, E], F32) for n in ("lg", "eq1", "lg2", "eq2", "c1", "cm")}
        r1 = {n: sb("r1_" + n, [128, 1], F32) for n in ("m1", "m2", "d", "e", "w1", "w2")}
        pp = [pst("pp%d" % i, [128, 512], F32) for i in range(2)]
        psS = [pst("psS%d" % i, [128, 512], F32) for i in range(2)]
        po = pst("po", [128, GS], F32)
        pd = pst("pd", [128, GS], F32)
        pvt = pst("pvt", [128, 128], BF16)

        S.op("vector", lambda e: e.memset(ones_b[:, :], 1.0), writes=["ones_b", ("ln1", "ones"), ("cln", "ones")])
        S.op("vector", lambda e: e.memset(ones_f[:, :], 1.0), writes=["ones_f"])
        S.op("vector", lambda e: e.memset(onesH[:, :], 1.0), writes=["onesH"])
        S.op("gpsimd", lambda e: e.affine_select(out=ident_b[:, :], in_=ones_b[:, :], pattern=[[-1, 128]], compare_op=ALU.is_equal,
                                                 fill=0.0, base=0, channel_multiplier=1),
             reads=["ones_b"], writes=["ident_b"])
        S.op("gpsimd", lambda e: e.affine_select(out=ident_f[:, :], in_=ones_f[0:H, 0:H], pattern=[[-1, H]], compare_op=ALU.is_equal,
                                                 fill=0.0, base=0, channel_multiplier=1),
             reads=["ones_f"], writes=["ident_f"])
        for h in range(H):
            S.op("gpsimd", lambda e, h=h: e.affine_select(out=sel[:, h, :], in_=ones_f[0:H, :], pattern=[[0, 128]], compare_op=ALU.is_equal,
                                                          fill=0.0, base=-h, channel_multiplier=1),
                 reads=["ones_f"], writes=["sel"])
        ones_gs = sb("ones_gs", [128, GS], BF16)
        S.op("vector", lambda e: e.memset(ones_gs[:, :], 1.0), writes=["ones_gs"])
        for i in range(TPG):
            S.op("gpsimd", lambda e, i=i: e.affine_select(out=dmask[i][:, :], in_=ones_gs[:, :], pattern=[[1, GS]],
                                                          compare_op=ALU.is_ge, fill=0.0, base=-128 * i, channel_multiplier=-1),
                 reads=["ones_gs"], writes=[("dmask", i)])
        S.dma(lambda e: e.dma_start(out=vect[:, :], in_=vec[:, :]), writes=["vect", ("ln1", "gb"), ("cln", "gb")])
        S.dma(lambda e: e.dma_start(out=bft[:, :], in_=bf[:, :]), writes=["bft"])
        S.dma(lambda e: e.dma_start(out=cwt[:, :], in_=cw[:, :]), writes=["cwt"])
        S.dma(lambda e: e.dma_start(out=wrt[:, :, :], in_=w_r.rearrange("(k p) e -> p k e", p=128)), writes=["wrt"])
        S.dma(lambda e: e.dma_start(out=brt[:, :], in_=b_r[:, :]), writes=["brt"])
        S.dma(lambda e: e.dma_start(out=flg[:, :], in_=flag[:, :]), writes=["flg"])
        S.op("vector", lambda e: e.tensor_scalar(out=mb[:, :], in0=flg[:, :], scalar1=BIG, scalar2=-BIG, op0=ALU.mult, op1=ALU.add),
             reads=["flg"], writes=["mb"])
        qb0 = VO["bm"] + off["q"] // 128
        S.op("vector", lambda e: e.tensor_scalar(out=bqs[:, :], in0=vect[:, qb0:qb0 + H], scalar1=QS, scalar2=None, op0=ALU.mult),
             reads=["vect"], writes=["bqs"])

        wctr = [0]

        def proj(W, kch, coff, size, act, tsl, n, out_ps, act_key, ps_key):
            p = wctr[0] % 2
            wctr[0] += 1
            S.dma(lambda e: e.dma_start(out=wf[p][:, 0:kch, 0:size], in_=W[:, coff:coff + size].rearrange("(k p) f -> p k f", p=128)),
                  writes=[("wf", p)])
            ceng = "gpsimd" if p == 0 else "vector"
            S.op(ceng, lambda e: e.tensor_copy(out=wb[p][:, 0:kch, 0:size], in_=wf[p][:, 0:kch, 0:size]),
                 reads=[("wf", p)], writes=[("wb", p)])
            for k in range(kch):
                S.op("tensor", lambda e, k=k: e.matmul(out_ps[0:size, 0:n], lhsT=wb[p][:, k, 0:size], rhs=act[:, k, tsl],
                                                      start=(k == 0), stop=(k == kch - 1)),
                     reads=[("wb", p)] + list(act_key), writes=[ps_key])

        def load_xb(src, g):
            for k in range(KD):
                q = k % 2
                S.dma(lambda e, k=k, q=q: e.dma_start(out=xs[q][:, :], in_=src[k * 128:(k + 1) * 128, g * GS:(g + 1) * GS]),
                      writes=[("xs", q)])
                S.op("vector", lambda e, k=k, q=q: e.tensor_copy(out=xb[:, k, :], in_=xs[q][:, :]),
                     reads=[("xs", q)], writes=["xb"])

        ppc = [0]

        def next_pp():
            i = ppc[0] % 2
            ppc[0] += 1
            return pp[i], ("pp", i)

        bcol = lambda name, j: vect[:, VO["bm"] + off[name] // 128 + j: VO["bm"] + off[name] // 128 + j + 1]
        gcol = lambda which, j: vect[:, VO["bm"] + off["f"] // 128 + which * KD + j: VO["bm"] + off["f"] // 128 + which * KD + j + 1]
        full = slice(0, GS)

        for t in range(NGA):
            src, g = (x_pre, t) if t < NGO else (x_own, t - NGO)
            load_xb(src, g)
            tcol = slice(t * GS, (t + 1) * GS)
            for h in range(H):
                ps_, pk = next_pp()
                proj(w_in, KD, off["k"] + h * 128, 128, xb, full, GS, ps_, ["xb"], pk)
                S.op("scalar", lambda e, h=h, ps_=ps_, tcol=tcol: e.activation(out=kT[:, h, tcol], in_=ps_[:, 0:GS], func=AF.Identity,
                                                                            bias=bcol("k", h), scale=1.0),
                     reads=[pk, "vect"], writes=[("kT", h)])
            for h in range(H):
                ps_, pk = next_pp()
                proj(w_in, KD, off["v"] + h * 128, 128, xb, full, GS, ps_, ["xb"], pk)
                S.op("scalar", lambda e, h=h, ps_=ps_: e.activation(out=vtmp[:, :], in_=ps_[:, 0:GS], func=AF.Identity,
                                                                   bias=bcol("v", h), scale=1.0),
                     reads=[pk, "vect"], writes=["vtmp"])
                for j in range(TPG):
                    kt = t * TPG + j
                    S.op("tensor", lambda e, j=j: e.transpose(pvt[:, :], vtmp[:, j * 128:(j + 1) * 128], ident_b[:, :]),
                         reads=["vtmp", "ident_b"], writes=["pvt"])
                    S.op("vector", lambda e, kt=kt, h=h: e.tensor_copy(out=V[:, kt, h * 128:(h + 1) * 128], in_=pvt[:, :]),
                         reads=["pvt"], writes=[("V", kt)])
            ps_, pk = next_pp()
            proj(w_in, KD, off["f"], H, xb, full, GS, ps_, ["xb"], pk)
            S.op("scalar", lambda e, ps_=ps_, tcol=tcol: e.activation(out=fT[:, tcol], in_=ps_[0:H, 0:GS], func=AF.Identity,
                                                                   bias=bft[:, 0:1], scale=1.0),
                 reads=[pk, "bft"], writes=["fT"])
            if t == NGO - 1:
                hs = slice(GS - 32, GS)
                for c in range(KC):
                    ps_, pk = next_pp()
                    proj(w_in, KD, off["a"] + c * 128, 128, xb, hs, 32, ps_, ["xb"], pk)
                    S.op("scalar", lambda e, c=c, ps_=ps_: e.activation(out=asb[:, 0:32], in_=ps_[:, 0:32], func=AF.Identity,
                                                                       bias=bcol("a", c), scale=1.0),
                         reads=[pk, "vect"], writes=["asb"])
                    ps2, pk2 = next_pp()
                    proj(w_in, KD, off["g"] + c * 128, 128, xb, hs, 32, ps2, ["xb"], pk2)
                    S.op("scalar", lambda e, c=c, ps2=ps2: e.activation(out=sgb[:, 0:32], in_=ps2[:, 0:32], func=AF.Sigmoid,
                                                                       bias=bcol("g", c), scale=1.0),
                         reads=[pk2, "vect"], writes=["sgb"])
                    S.op("vector", lambda e, c=c: e.tensor_tensor(out=uhalo[:, c, :], in0=asb[:, 0:32], in1=sgb[:, 0:32], op=ALU.mult),
                         reads=["asb", "sgb"], writes=["uhalo"])
                    S.op("vector", lambda e, c=c: e.tensor_scalar(out=uhalo[:, c, :], in0=uhalo[:, c, :], scalar1=flg[:, 0:1], scalar2=None, op0=ALU.mult),
                         reads=["uhalo", "flg"], writes=["uhalo"])

        S.op("vector", lambda e: e.tensor_scalar(out=f2[:, :], in0=fT[:, :], scalar1=-1.0, scalar2=None, op0=ALU.mult),
             reads=["fT"], writes=["f2"])
        S.op("vector", lambda e: e.tensor_tensor(out=f2[:, :], in0=f2[:, :], in1=fT[:, :], op=ALU.max),
             reads=["fT", "f2"], writes=["f2"])
        S.op("scalar", lambda e: e.activation(out=f2[:, :], in_=f2[:, :], func=AF.Exp, scale=-1.0), reads=["f2"], writes=["f2"])
        S.op("scalar", lambda e: e.activation(out=f2[:, :], in_=f2[:, :], func=AF.Ln, bias=1.0, scale=1.0), reads=["f2"], writes=["f2"])
        S.op("vector", lambda e: e.tensor_scalar(out=fT[:, :], in0=fT[:, :], scalar1=0.0, scalar2=None, op0=ALU.min),
             reads=["fT"], writes=["fT"])
        S.op("vector", lambda e: e.tensor_tensor(out=fT[:, :], in0=fT[:, :], in1=f2[:, :], op=ALU.subtract),
             reads=["fT", "f2"], writes=["fT"])
        S.op("vector", lambda e: e.tensor_tensor_scan(out=cumT[:, :], data0=onesH[:, :], data1=fT[:, :], initial=0.0,
                                                      op0=ALU.mult, op1=ALU.add),
             reads=["fT", "onesH"], writes=["cumT"])
        for kt in range(NKT):
            ps_, pk = next_pp()
            S.op("tensor", lambda e, kt=kt, ps_=ps_: e.transpose(ps_[:, 0:H], cumT[:, kt * 128:(kt + 1) * 128], ident_f[:, :]),
                 reads=["cumT", "ident_f"], writes=[pk])
            if kt < NKT // 2:
                S.op("vector", lambda e, kt=kt, ps_=ps_: e.tensor_scalar(out=nbias[:, kt, :], in0=ps_[:, 0:H], scalar1=-1.0, scalar2=mb[:, 0:1],
                                                                        op0=ALU.mult, op1=ALU.add),
                     reads=[pk, "mb"], writes=["nbias"])
            else:
                S.op("vector", lambda e, kt=kt, ps_=ps_: e.tensor_scalar(out=nbias[:, kt, :], in0=ps_[:, 0:H], scalar1=-1.0, scalar2=None, op0=ALU.mult),
                     reads=[pk], writes=["nbias"])

        ukeys = [("u", c) for c in range(KC)] + [("y", c) for c in range(KC)] + [("cln", "v", c) for c in range(KC)] + [("cln", "of", c) for c in range(KC)]
        vkeys = [("ln1", "v", k) for k in range(KD)] + [("ln1", "of", k) for k in range(KD)]
        sti = [0]
        for gi in range(NGO):
            load_xb(x_own, gi)
            gq = slice(TL + gi * GS, TL + (gi + 1) * GS)
            for c in range(KC):
                ps_, pk = next_pp()
                proj(w_in, KD, off["a"] + c * 128, 128, xb, full, GS, ps_, ["xb"], pk)
                S.op("scalar", lambda e, c=c, ps_=ps_: e.activation(out=asb[:, :], in_=ps_[:, 0:GS], func=AF.Identity, bias=bcol("a", c), scale=1.0),
                     reads=[pk, "vect"], writes=["asb"])
                ps2, pk2 = next_pp()
                proj(w_in, KD, off["g"] + c * 128, 128, xb, full, GS, ps2, ["xb"], pk2)
                S.op("scalar", lambda e, c=c, ps2=ps2: e.activation(out=sgb[:, :], in_=ps2[:, 0:GS], func=AF.Sigmoid, bias=bcol("g", c), scale=1.0),
                     reads=[pk2, "vect"], writes=["sgb"])
                S.op("vector", lambda e, c=c: e.tensor_tensor(out=u[:, c, 32:32 + GS], in0=asb[:, :], in1=sgb[:, :], op=ALU.mult),
                     reads=["asb", "sgb"], writes=[("u", c)] + (vkeys if c == 0 else []))
                hsrc = uhalo if gi == 0 else utail
                S.op("vector", lambda e, c=c, hsrc=hsrc: e.tensor_copy(out=u[:, c, 0:32], in_=hsrc[:, c, :]),
                     reads=["uhalo", "utail"], writes=[("u", c)])
                ceng = "vector"
                for j in range(CONV_K):
                    wcol = cwt[:, c * CONV_K + j: c * CONV_K + j + 1]
                    src = u[:, c, 2 + j: 2 + j + GS]
                    if j == 0:
                        S.op(ceng, lambda e, c=c, wcol=wcol, src=src: e.tensor_scalar(out=y[:, c, :], in0=src, scalar1=wcol,
                                                                                    scalar2=vect[:, VO["cb"] + c: VO["cb"] + c + 1], op0=ALU.mult, op1=ALU.add),
                             reads=[("u", c), "cwt", "vect"], writes=[("y", c), ("cln", "v", c)])
                    else:
                        S.op(ceng, lambda e, c=c, wcol=wcol, src=src: e.scalar_tensor_tensor(out=y[:, c, :], in0=src, scalar=wcol, in1=y[:, c, :],
                                                                                           op0=ALU.mult, op1=ALU.add),
                             reads=[("u", c), "cwt", ("y", c)], writes=[("y", c), ("cln", "v", c)])
                S.op("vector", lambda e, c=c: e.tensor_copy(out=utail[:, c, :], in_=u[:, c, GS:GS + 32]),
                     reads=[("u", c)], writes=["utail"])
            _ln_feature_major(S, nc, y, KC, GS, vect, VO["cg"], VO["cbeta"], y, None, scr, psS, ones_b, "cln", ps_keys=("psS0", "psS1"))
            for c in range(KC):
                S.op("scalar", lambda e, c=c: e.activation(out=z[:, c, :], in_=y[:, c, :], func=AF.Silu),
                     reads=[("cln", "of", c)], writes=[("z", c)])
            for h in range(H):
                ps_, pk = next_pp()
                proj(w_in, KD, off["q"] + h * 128, 128, xb, full, GS, ps_, ["xb"], pk)
                S.op("scalar", lambda e, h=h, ps_=ps_: e.activation(out=qT[:, h, :], in_=ps_[:, 0:GS], func=AF.Identity, bias=bqs[:, h:h + 1], scale=QS),
                     reads=[pk, "bqs"], writes=[("qT", h)])
            nkt_g = NKT // 2 + (gi + 1) * TPG
            for h in range(H):
                for kt in range(nkt_g):
                    sp = kt % 2
                    S.op("tensor", lambda e, h=h, kt=kt, sp=sp: e.matmul(psS[sp][:, 0:GS], lhsT=kT[:, h, kt * 128:(kt + 1) * 128], rhs=qT[:, h, :],
                                                                        start=True, stop=False),
                         reads=[("kT", h), ("qT", h)], writes=["psS%d" % sp])
                    S.op("tensor", lambda e, h=h, sp=sp, gq=gq: e.matmul(psS[sp][:, 0:GS], lhsT=sel[:, h, :], rhs=cumT[:, gq], start=False, stop=True),
                         reads=["sel", "cumT"], writes=["psS%d" % sp])
                    S.op("scalar", lambda e, h=h, kt=kt, sp=sp: e.activation(out=PT[sp][:, :], in_=psS[sp][:, 0:GS], func=AF.Exp,
                                                                            bias=nbias[:, kt, h:h + 1], scale=1.0),
                         reads=["psS%d" % sp, "nbias"], writes=[("PT", sp)])
                    dj = kt - (NKT // 2 + gi * TPG)
                    if dj >= 0:
                        S.op("vector", lambda e, sp=sp, dj=dj: e.tensor_tensor(out=PT[sp][:, :], in0=PT[sp][:, :], in1=dmask[dj][:, :], op=ALU.mult),
                             reads=[("PT", sp), ("dmask", dj)], writes=[("PT", sp)])
                    S.op("tensor", lambda e, h=h, kt=kt, sp=sp, nkt_g=nkt_g: e.matmul(po[:, 0:GS], lhsT=V[:, kt, h * 128:(h + 1) * 128], rhs=PT[sp][:, :],
                                                                        start=(kt == 0), stop=(kt == nkt_g - 1)),
                         reads=[("V", kt), ("PT", sp)], writes=["po"])
                    S.op("tensor", lambda e, kt=kt, sp=sp, nkt_g=nkt_g: e.matmul(pd[:, 0:GS], lhsT=ones_b[:, :], rhs=PT[sp][:, :],
                                                                   start=(kt == 0), stop=(kt == nkt_g - 1)),
                         reads=["ones_b", ("PT", sp)], writes=["pd"])
                S.op("vector", lambda e: e.reciprocal(out=rden[:, :], in_=pd[:, 0:GS]), reads=["pd"], writes=["rden"])
                S.op("vector", lambda e, h=h: e.tensor_tensor(out=OT[:, h, :], in0=po[:, 0:GS], in1=rden[:, :], op=ALU.mult),
                     reads=["po", "rden"], writes=[("OT", h)])
            for n in range(KD):
                ps_, pk = next_pp()
                proj(w_in, KD, off["gc"] + n * 128, 128, xb, full, GS, ps_, ["xb"], pk)
                S.op("scalar", lambda e, n=n, ps_=ps_: e.activation(out=gcb[:, :], in_=ps_[:, 0:GS], func=AF.Sigmoid, bias=gcol(0, n), scale=1.0),
                     reads=[pk, "vect"], writes=["gcb"])
                ps_, pk = next_pp()
                proj(w_in, KD, off["ga"] + n * 128, 128, xb, full, GS, ps_, ["xb"], pk)
                S.op("scalar", lambda e, n=n, ps_=ps_: e.activation(out=gab[:, :], in_=ps_[:, 0:GS], func=AF.Sigmoid, bias=gcol(1, n), scale=1.0),
                     reads=[pk, "vect"], writes=["gab"])
                ps_, pk = next_pp()
                proj(w_co, KC, n * 128, 128, z, full, GS, ps_, [("z", c) for c in range(KC)], pk)
                S.op("scalar", lambda e, n=n, ps_=ps_: e.activation(out=ycb[:, :], in_=ps_[:, 0:GS], func=AF.Identity,
                                                                   bias=vect[:, VO["bco"] + n: VO["bco"] + n + 1], scale=1.0),
                     reads=[pk, "vect"], writes=["ycb"])
                S.op("vector", lambda e: e.tensor_tensor(out=gcb[:, :], in0=gcb[:, :], in1=ycb[:, :], op=ALU.mult),
                     reads=["gcb", "ycb"], writes=["gcb"])
                ps_, pk = next_pp()
                proj(w_ao, H, n * 128, 128, OT, full, GS, ps_, [("OT", h) for h in range(H)], pk)
                S.op("vector", lambda e, ps_=ps_: e.tensor_tensor(out=gab[:, :], in0=gab[:, :], in1=ps_[:, 0:GS], op=ALU.mult),
                     reads=["gab", pk], writes=["gab"])
                S.op("vector", lambda e, n=n: e.tensor_tensor(out=mT[:, n, :], in0=gcb[:, :], in1=gab[:, :], op=ALU.add),
                     reads=["gcb", "gab"], writes=[("mT", n)])
            for n in range(KD):
                ps_, pk = next_pp()
                proj(w_o, KD, n * 128, 128, mT, full, GS, ps_, [("mT", k) for k in range(KD)], pk)
                q = n % 2
                S.dma(lambda e, n=n, q=q, gi=gi: e.dma_start(out=xs[q][:, :], in_=x_own[n * 128:(n + 1) * 128, gi * GS:(gi + 1) * GS]),
                      writes=[("xs", q)])
                S.op("vector", lambda e, n=n, q=q, ps_=ps_: e.scalar_tensor_tensor(out=v[:, n, :], in0=xs[q][:, :], scalar=float(ALPHA), in1=ps_[:, 0:GS],
                                                                                 op0=ALU.mult, op1=ALU.add),
                     reads=[("xs", q), pk], writes=[("ln1", "v", n)] + (ukeys if n == 0 else []))
            _ln_feature_major(S, nc, v, KD, GS, vect, VO["l1g"], VO["l1b"], v, None, scr, psS, ones_b, "ln1", ps_keys=("psS0", "psS1"))
            gsl = slice(gi * GS, (gi + 1) * GS)
            for n in range(KD):
                S.dma(lambda e, n=n, gsl=gsl: e.dma_start(out=x1T[n * 128:(n + 1) * 128, gsl], in_=v[:, n, :]),
                      reads=[("ln1", "of", n)], writes=[("x1T", n, gi)])
                q = sti[0] % 2
                sti[0] += 1
                S.op("scalar", lambda e, n=n, q=q: e.activation(out=stb[q][:, :], in_=v[:, n, :], func=AF.Copy),
                     reads=[("ln1", "of", n)], writes=[("stb", q)])
                S.dma(lambda e, n=n, q=q, gsl=gsl: e.dma_start(out=x1Tb[n * 128:(n + 1) * 128, gsl], in_=stb[q][:, :]),
                      reads=[("stb", q)], writes=[("x1Tb", n, gi)])
            for j in range(TPG):
                ps_, pk = next_pp()
                for k in range(KD):
                    S.op("tensor", lambda e, k=k, j=j, ps_=ps_: e.matmul(ps_[:, 0:E], lhsT=v[:, k, j * 128:(j + 1) * 128], rhs=wrt[:, k, :],
                                                                        start=(k == 0), stop=False),
                         reads=[("ln1", "of", k), "wrt"], writes=[pk])
                S.op("tensor", lambda e, ps_=ps_: e.matmul(ps_[:, 0:E], lhsT=ones_f[0:1, :], rhs=brt[:, :], start=False, stop=True),
                     reads=["ones_f", "brt"], writes=[pk])
                R, r = rt, r1
                S.op("vector", lambda e, ps_=ps_: e.tensor_copy(out=R["lg"][:, :], in_=ps_[:, 0:E]), reads=[pk], writes=["r_lg"])
                S.op("vector", lambda e: e.reduce_max(out=r["m1"][:, :], in_=R["lg"][:, :], axis=mybir.AxisListType.X), reads=["r_lg"], writes=["r_m1"])
                S.op("vector", lambda e: e.tensor_scalar(out=R["eq1"][:, :], in0=R["lg"][:, :], scalar1=r["m1"][:, 0:1], scalar2=None, op0=ALU.is_equal),
                     reads=["r_lg", "r_m1"], writes=["r_eq1"])
                S.op("vector", lambda e: e.scalar_tensor_tensor(out=R["lg2"][:, :], in0=R["eq1"][:, :], scalar=-BIG, in1=R["lg"][:, :], op0=ALU.mult, op1=ALU.add),
                     reads=["r_eq1", "r_lg"], writes=["r_lg2"])
                S.op("vector", lambda e: e.reduce_max(out=r["m2"][:, :], in_=R["lg2"][:, :], axis=mybir.AxisListType.X), reads=["r_lg2"], writes=["r_m2"])
                S.op("vector", lambda e: e.tensor_scalar(out=R["eq2"][:, :], in0=R["lg2"][:, :], scalar1=r["m2"][:, 0:1], scalar2=None, op0=ALU.is_equal),
                     reads=["r_lg2", "r_m2"], writes=["r_eq2"])
                S.op("vector", lambda e: e.tensor_tensor(out=r["d"][:, :], in0=r["m2"][:, :], in1=r["m1"][:, :], op=ALU.subtract),
                     reads=["r_m1", "r_m2"], writes=["r_d"])
                S.op("scalar", lambda e: e.activation(out=r["e"][:, :], in_=r["d"][:, :], func=AF.Exp), reads=["r_d"], writes=["r_e"])
                S.op("vector", lambda e: e.tensor_scalar(out=r["w1"][:, :], in0=r["e"][:, :], scalar1=1.0, scalar2=None, op0=ALU.add),
                     reads=["r_e"], writes=["r_w1"])
                S.op("vector", lambda e: e.reciprocal(out=r["w1"][:, :], in_=r["w1"][:, :]), reads=["r_w1"], writes=["r_w1"])
                S.op("vector", lambda e: e.tensor_tensor(out=r["w2"][:, :], in0=r["e"][:, :], in1=r["w1"][:, :], op=ALU.mult),
                     reads=["r_e", "r_w1"], writes=["r_w2"])
                S.op("vector", lambda e: e.tensor_scalar(out=R["c1"][:, :], in0=R["eq1"][:, :], scalar1=r["w1"][:, 0:1], scalar2=None, op0=ALU.mult),
                     reads=["r_eq1", "r_w1"], writes=["r_c1"])
                S.op("vector", lambda e: e.scalar_tensor_tensor(out=R["cm"][:, :], in0=R["eq2"][:, :], scalar=r["w2"][:, 0:1], in1=R["c1"][:, :],
                                                                op0=ALU.mult, op1=ALU.add),
                     reads=["r_eq2", "r_w2", "r_c1"], writes=["r_cm"])
                t0 = gi * GS + j * 128
                S.dma(lambda e, t0=t0: e.dma_start(out=comb[t0:t0 + 128, :], in_=R["cm"][:, :]), reads=["r_cm"], writes=[("comb", t0)])
        S.emit(nc, st)
    return nc


_PROGS = {}


def _prog(key, fn):
    if key not in _PROGS:
        _PROGS[key] = fn()
    return _PROGS[key]


def _launch(nc, in_maps):
    res = run_bass_kernel_spmd(nc, in_maps, core_ids=list(range(NCORES)))
    return res.results


def _pk(v):
    return np.ascontiguousarray(np.asarray(v, np.float32).reshape(-1, 128).T)


def kernel(x, w_in, b_in, conv_w, conv_b, conv_ln_g, conv_ln_b, w_conv_out, b_conv_out,
           w_attn_out, w_o, ln1_g, ln1_b, ffn_wg, ffn_wu, ffn_wd, router_w, router_b,
           exp_wg, exp_wu, exp_wd, ln2_g, ln2_b):
    D, H, E, TL = D_MODEL, N_HEADS, N_EXPERTS, SEQ // 2
    T = BATCH * SEQ
    CC, A, off = mix_dims(D, H, E)
    KC = CC // 128
    x = np.asarray(x, np.float32)
    mix = _prog("mix", lambda: build_mix(D, TL, H, E))
    cmb = _prog("comb", lambda: build_combine(D, TL, NCORES))
    FS = D_FF // NCORES
    ffn_d = _prog("ffn_d", lambda: build_ffn(D, T, FS))
    ffn_e = _prog("ffn_e", lambda: build_ffn(D, T, D_FF))
    xcur = [np.ascontiguousarray(x[c // 2, (c % 2) * TL:(c % 2 + 1) * TL, :].T) for c in range(NCORES)]
    zeros_pre = np.zeros((D, TL), np.float32)
    ones_comb = np.ones((1, T), np.float32)
    for l in range(DEPTH):
        j = l // 2
        b = np.asarray(b_in[l], np.float32)
        bm = np.concatenate([b[:off["f"]], b[off["gc"]:]])
        vec = np.concatenate([_pk(bm), _pk(conv_b[l]), _pk(conv_ln_g[l]), _pk(conv_ln_b[l]), _pk(b_conv_out[l]),
                              _pk(ln1_g[l]), _pk(ln1_b[l])], axis=1)
        cw = np.ascontiguousarray(np.asarray(conv_w[l], np.float32).T.reshape(KC, 128, CONV_K).transpose(1, 0, 2).reshape(128, KC * CONV_K))
        bfv = np.ascontiguousarray(b[off["f"]:off["gc"]].reshape(H, 1))
        maps = []
        for c in range(NCORES):
            odd = c % 2 == 1
            maps.append({"x_own": xcur[c], "x_pre": xcur[c - 1] if odd else zeros_pre,
                         "w_in": np.asarray(w_in[l], np.float32), "w_co": np.asarray(w_conv_out[l], np.float32),
                         "w_ao": np.asarray(w_attn_out[l], np.float32), "w_o": np.asarray(w_o[l], np.float32),
                         "vec": vec, "bf": bfv, "cw": cw, "w_r": np.asarray(router_w[j], np.float32),
                         "b_r": np.asarray(router_b[j], np.float32).reshape(1, E),
                         "flag": np.full((128, 1), 1.0 if odd else 0.0, np.float32)})
        r = _launch(mix, maps)
        x1T = [np.asarray(r[c]["x1T"]) for c in range(NCORES)]
        xall = np.ascontiguousarray(np.concatenate([np.asarray(r[c]["x1Tb"]) for c in range(NCORES)], axis=1))
        maps = []
        if l % 2 == 0:
            for e in range(NCORES):
                g_, u_, d_ = ffn_weight_layout(ffn_wg[j][:, e * FS:(e + 1) * FS], ffn_wu[j][:, e * FS:(e + 1) * FS],
                                               ffn_wd[j][e * FS:(e + 1) * FS, :])
                maps.append({"xT": xall, "comb": ones_comb, "wg": g_, "wu": u_, "wd": d_})
            r = _launch(ffn_d, maps)
        else:
            call = np.concatenate([np.asarray(r[c]["comb"]) for c in range(NCORES)], axis=0)
            for e in range(NCORES):
                g_, u_, d_ = ffn_weight_layout(exp_wg[j][e], exp_wu[j][e], exp_wd[j][e])
                maps.append({"xT": xall, "comb": np.ascontiguousarray(call[:, e].reshape(1, T)), "wg": g_, "wu": u_, "wd": d_})
            r = _launch(ffn_e, maps)
        yT = [np.asarray(r[e]["yT"]) for e in range(NCORES)]
        gb = np.concatenate([_pk(ln2_g[l]), _pk(ln2_b[l])], axis=1)
        maps = []
        for c in range(NCORES):
            parts = np.ascontiguousarray(np.concatenate([yT[e][:, c * TL:(c + 1) * TL] for e in range(NCORES)], axis=0))
            maps.append({"x1T": x1T[c], "parts": parts, "gb": gb})
        r = _launch(cmb, maps)
        xcur = [np.asarray(r[c]["xo"]) for c in range(NCORES)]
    out = np.empty((BATCH, SEQ, D), np.float32)
    for c in range(NCORES):
        out[c // 2, (c % 2) * TL:(c % 2 + 1) * TL, :] = xcur[c].T
    return out
```

```python
import numpy as np
import ml_dtypes
import concourse.bass as bass
import concourse.mybir as mybir
from concourse.bass_utils import run_bass_kernel_spmd

F32 = mybir.dt.float32
BF16 = mybir.dt.bfloat16
AF = mybir.ActivationFunctionType
ALU = mybir.AluOpType

D_MODEL = 2048
BATCH = 4
SEQ = 2048
DEPTH = 4
N_HEADS = 8
HEAD_DIM = 128
CONV_K = 31
D_FF = 5632
N_EXPERTS = 8
LN_EPS = 1e-5
ALPHA = (2 * DEPTH) ** 0.25
NCORES = 8


class Sched:
    ENGS = ("tensor", "scalar", "vector", "gpsimd", "sync")
    NSTREAM = 8
    NGSTREAM = 6

    def __init__(self):
        self.ops = {e: [] for e in self.ENGS}
        self.cnt = {}
        self.last_w = {}
        self.readers = {}
        self.waited = {e: {} for e in self.ENGS}
        self.stream_rr = 0
        self.gstream_rr = 0
        self.out_dmas = []

    def _deps(self, reads, writes):
        deps = []
        for b in reads:
            if b in self.last_w:
                deps.append(self.last_w[b])
        for b in writes:
            if b in self.last_w:
                deps.append(self.last_w[b])
            deps.extend(self.readers.get(b, ()))
        return deps

    def _commit(self, tok, reads, writes):
        for b in reads:
            self.readers.setdefault(b, []).append(tok)
        for b in writes:
            self.last_w[b] = tok
            self.readers[b] = []

    def _filter(self, eng, deps):
        best = {}
        for s, v in deps:
            if v > best.get(s, 0):
                best[s] = v
        out = []
        w = self.waited[eng]
        for s, v in best.items():
            if w.get(s, 0) < v:
                w[s] = v
                out.append((s, v))
        return out

    def op(self, eng, fn, reads=(), writes=()):
        sem = "c_" + eng
        deps = self._deps(reads, writes)
        val = self.cnt.get(sem, 0) + 1
        self.cnt[sem] = val
        self.ops[eng].append((self._filter(eng, deps), fn, sem, 1))
        self._commit((sem, val), reads, writes)

    def dma(self, fn, reads=(), writes=(), eng="sync", is_out=False):
        if eng == "gpsimd":
            st = self.gstream_rr
            self.gstream_rr = (st + 1) % self.NGSTREAM
            sem = "g_%d" % st
        else:
            st = self.stream_rr
            self.stream_rr = (st + 1) % self.NSTREAM
            sem = "d_%d" % st
        deps = self._deps(reads, writes)
        prev = self.cnt.get(sem, 0)
        if prev:
            deps.append((sem, prev))
        val = prev + 16
        self.cnt[sem] = val
        self.ops[eng].append((self._filter(eng, deps), fn, sem, 16))
        self._commit((sem, val), reads, writes)

    def emit(self, nc, stack):
        names = ["c_" + e for e in self.ENGS if e != "sync"] + ["d_%d" % i for i in range(self.NSTREAM)] + ["g_%d" % i for i in range(self.NGSTREAM)]
        sems = {n: stack.enter_context(nc.semaphore(n)) for n in names}
        finals = [(s, v) for s, v in self.cnt.items() if v > 0]
        block = stack.enter_context(nc.Block())
        ops = self.ops

        def run(eng_obj, lst, final=False):
            for waits, fn, sem, inc in lst:
                for s, v in waits:
                    eng_obj.wait_ge(sems[s], v)
                ins = fn(eng_obj)
                ins.then_inc(sems[sem], inc)
            if final:
                for s, v in finals:
                    eng_obj.wait_ge(sems[s], v)

        @block.tensor
        def _(e):
            run(e, ops["tensor"])

        @block.scalar
        def _(e):
            run(e, ops["scalar"])

        @block.vector
        def _(e):
            run(e, ops["vector"])

        @block.gpsimd
        def _(e):
            run(e, ops["gpsimd"])

        @block.sync
        def _(e):
            run(e, ops["sync"], final=True)


def _chunks(n):
    out = []
    o = 0
    while o < n:
        s = min(128, n - o)
        out.append((o, s))
        o += s
    return out


def _ln_feature_major(S, nc, v, nk, ntok, gbt, goff, boff, out_f32, out_bf, scr, ps, ones_b, tag, ps_keys=None):
    pk0, pk1 = ps_keys if ps_keys else ((tag, 'ps0'), (tag, 'ps1'))
    nfeat = float(nk * 128)
    for g0 in range(0, ntok, 512):
        n = min(512, ntok - g0)
        sl = slice(g0, g0 + n)
        for k in range(nk):
            S.op("vector", lambda e, k=k, n=n, sl=sl: e.tensor_copy(out=scr["b1"][:, 0:n], in_=v[:, k, sl]),
                 reads=[(tag, "v", k)], writes=[(tag, "b1")])
            S.op("scalar", lambda e, k=k, n=n, sl=sl: e.activation(out=scr["b2"][:, 0:n], in_=v[:, k, sl], func=AF.Square),
                 reads=[(tag, "v", k)], writes=[(tag, "b2")])
            S.op("tensor", lambda e, k=k, n=n, sl=sl: e.matmul(ps[0][:, 0:n], lhsT=ones_b[:, :], rhs=scr["b1"][:, 0:n],
                                                  start=(k == 0), stop=(k == nk - 1)),
                 reads=[(tag, "b1"), (tag, "ones")], writes=[pk0])
            S.op("tensor", lambda e, k=k, n=n, sl=sl: e.matmul(ps[1][:, 0:n], lhsT=ones_b[:, :], rhs=scr["b2"][:, 0:n],
                                                  start=(k == 0), stop=(k == nk - 1)),
                 reads=[(tag, "b2"), (tag, "ones")], writes=[pk1])
        S.op("scalar", lambda e, n=n, sl=sl: e.activation(out=scr["mean"][:, 0:n], in_=ps[0][:, 0:n], func=AF.Copy, scale=1.0 / nfeat),
             reads=[pk0], writes=[(tag, "mean")])
        S.op("vector", lambda e, n=n, sl=sl: e.tensor_tensor(out=scr["t"][:, 0:n], in0=scr["mean"][:, 0:n], in1=scr["mean"][:, 0:n], op=ALU.mult),
             reads=[(tag, "mean")], writes=[(tag, "t")])
        S.op("vector", lambda e, n=n, sl=sl: e.scalar_tensor_tensor(out=scr["rstd"][:, 0:n], in0=ps[1][:, 0:n], scalar=1.0 / nfeat,
                                                        in1=scr["t"][:, 0:n], op0=ALU.mult, op1=ALU.subtract),
             reads=[pk1, (tag, "t")], writes=[(tag, "rstd")])
        S.op("vector", lambda e, n=n, sl=sl: e.tensor_scalar(out=scr["rstd"][:, 0:n], in0=scr["rstd"][:, 0:n], scalar1=0.0, scalar2=LN_EPS,
                                                 op0=ALU.max, op1=ALU.add),
             reads=[(tag, "rstd")], writes=[(tag, "rstd")])
        S.op("scalar", lambda e, n=n, sl=sl: e.activation(out=scr["rstd"][:, 0:n], in_=scr["rstd"][:, 0:n], func=AF.Sqrt),
             reads=[(tag, "rstd")], writes=[(tag, "rstd")])
        S.op("vector", lambda e, n=n, sl=sl: e.reciprocal(out=scr["rstd"][:, 0:n], in_=scr["rstd"][:, 0:n]),
             reads=[(tag, "rstd")], writes=[(tag, "rstd")])
        for k in range(nk):
            S.op("vector", lambda e, k=k, n=n, sl=sl: e.tensor_tensor(out=scr["t"][:, 0:n], in0=v[:, k, sl], in1=scr["mean"][:, 0:n], op=ALU.subtract),
                 reads=[(tag, "v", k), (tag, "mean")], writes=[(tag, "t")])
            S.op("vector", lambda e, k=k, n=n, sl=sl: e.tensor_tensor(out=scr["t"][:, 0:n], in0=scr["t"][:, 0:n], in1=scr["rstd"][:, 0:n], op=ALU.mult),
                 reads=[(tag, "t"), (tag, "rstd")], writes=[(tag, "t")])
            if out_f32 is not None:
                S.op("scalar", lambda e, k=k, n=n, sl=sl: e.activation(out=out_f32[:, k, sl], in_=scr["t"][:, 0:n], func=AF.Identity,
                                                          scale=gbt[:, goff + k:goff + k + 1], bias=gbt[:, boff + k:boff + k + 1]),
                     reads=[(tag, "t"), (tag, "gb")], writes=[(tag, "of", k)])
            if out_bf is not None:
                S.op("scalar", lambda e, k=k, n=n, sl=sl: e.activation(out=out_bf[:, k, sl], in_=scr["t"][:, 0:n], func=AF.Identity,
                                                          scale=gbt[:, goff + k:goff + k + 1], bias=gbt[:, boff + k:boff + k + 1]),
                     reads=[(tag, "t"), (tag, "gb")], writes=[(tag, "ob", k)])


def ffn_weight_layout(wg, wu, wd):
    D, FL = wg.shape
    KD = D // 128
    NF = (FL + 127) // 128
    FP = NF * 128

    def up(w):
        wp = np.zeros((D, FP), np.float32)
        wp[:, :FL] = w
        return np.ascontiguousarray(wp.reshape(KD, 128, NF, 128).transpose(2, 1, 0, 3))

    dp = np.zeros((FP, D), np.float32)
    dp[:FL] = wd
    wdp = np.ascontiguousarray(dp.reshape(NF, 128, KD, 128).transpose(2, 1, 0, 3))
    return up(wg), up(wu), wdp


def build_ffn(D, T, FL, TB=1024):
    from contextlib import ExitStack
    KD = D // 128
    fch = _chunks(FL)
    NF = len(fch)
    NG = TB // 512
    nc = bass.Bass("TRN2", target_bir_lowering=False)
    xT = nc.dram_tensor("xT", [D, T], BF16, kind="ExternalInput").ap()
    comb = nc.dram_tensor("comb", [1, T], F32, kind="ExternalInput").ap()
    wg = nc.dram_tensor("wg", [NF, 128, KD, 128], F32, kind="ExternalInput").ap()
    wu = nc.dram_tensor("wu", [NF, 128, KD, 128], F32, kind="ExternalInput").ap()
    wd = nc.dram_tensor("wd", [KD, 128, NF, 128], F32, kind="ExternalInput").ap()
    yT = nc.dram_tensor("yT", [D, T], F32, kind="ExternalOutput").ap()
    S = Sched()
    with ExitStack() as st:
        sb = lambda name, shape, dt: st.enter_context(nc.sbuf_tensor(name, shape, dt))
        xb = sb("xb", [128, KD, TB], BF16)
        hT = sb("hT", [128, NF, TB], BF16)
        cb1 = sb("cb1", [1, TB], F32)
        ones1 = sb("ones1", [1, 128], F32)
        comb_bc = sb("comb_bc", [128, TB], F32)
        NWB = 3
        wgb = [sb("wgb%d" % i, [128, KD, 128], BF16) for i in range(NWB)]
        wub = [sb("wub%d" % i, [128, KD, 128], BF16) for i in range(NWB)]
        wdb = [sb("wdb%d" % i, [128, NF, 128], BF16) for i in range(2)]
        sg = [sb("sg%d" % i, [128, 512], F32) for i in range(2)]
        yo = [sb("yo%d" % i, [128, 512], F32) for i in range(2)]
        ps = [st.enter_context(nc.psum_tensor("ps%d" % i, [128, 512], F32)) for i in range(8)]

        S.op("vector", lambda e: e.memset(ones1[:, :], 1.0), writes=["ones1"])
        ci = 0
        di = 0
        ei = 0
        for b in range(T // TB):
            t0 = b * TB
            S.dma(lambda e, t0=t0: e.dma_start(out=xb[:, :, :], in_=xT[:, t0:t0 + TB].rearrange("(k p) t -> p k t", p=128)),
                  writes=["xb"])
            S.dma(lambda e, t0=t0: e.dma_start(out=cb1[:, :], in_=comb[:, t0:t0 + TB]), writes=["cb1"])
            for g in range(NG):
                gs = slice(g * 512, (g + 1) * 512)
                S.op("tensor", lambda e, gs=gs: e.matmul(ps[0][:, :], lhsT=ones1[:, :], rhs=cb1[:, gs], start=True, stop=True),
                     reads=["ones1", "cb1"], writes=[("ps", 0)])
                S.op("scalar", lambda e, gs=gs: e.activation(out=comb_bc[:, gs], in_=ps[0][:, :], func=AF.Copy),
                     reads=[("ps", 0)], writes=["comb_bc"])
            for fi, (fo, fs) in enumerate(fch):
                p = ci % NWB
                q = ci % 2
                ci += 1
                S.dma(lambda e, p=p, fi=fi: e.dma_start(out=wgb[p][:, :, :], in_=wg[fi, :, :, :]), writes=[("wgb", p)], eng="gpsimd")
                S.dma(lambda e, p=p, fi=fi: e.dma_start(out=wub[p][:, :, :], in_=wu[fi, :, :, :]), writes=[("wub", p)], eng="gpsimd")
                for g in range(NG):
                    gs = slice(g * 512, (g + 1) * 512)
                    bg, bu = 4 * q + 2 * g, 4 * q + 2 * g + 1

                    def mm_g(e, p=p, fs=fs, gs=gs, bg=bg):
                        for k in range(KD):
                            ins = e.matmul(ps[bg][0:fs, :], lhsT=wgb[p][:, k, 0:fs], rhs=xb[:, k, gs], start=(k == 0), stop=(k == KD - 1))
                        return ins

                    def mm_u(e, p=p, fs=fs, gs=gs, bu=bu):
                        for k in range(KD):
                            ins = e.matmul(ps[bu][0:fs, :], lhsT=wub[p][:, k, 0:fs], rhs=xb[:, k, gs], start=(k == 0), stop=(k == KD - 1))
                        return ins

                    S.op("tensor", mm_g, reads=[("wgb", p), "xb"], writes=[("ps", bg)])
                    S.op("tensor", mm_u, reads=[("wub", p), "xb"], writes=[("ps", bu)])
                    r = ei % 2
                    ei += 1
                    S.op("scalar", lambda e, r=r, fs=fs, bg=bg: e.activation(out=sg[r][0:fs, :], in_=ps[bg][0:fs, :], func=AF.Silu),
                         reads=[("ps", bg)], writes=[("sg", r)])
                    S.op("vector", lambda e, r=r, fs=fs, fi=fi, gs=gs, bu=bu: e.tensor_tensor(out=hT[0:fs, fi, gs], in0=sg[r][0:fs, :], in1=ps[bu][0:fs, :], op=ALU.mult),
                         reads=[("sg", r), ("ps", bu)], writes=[("hT", fi, g)])
            for n in range(KD):
                p = di % 2
                S.dma(lambda e, p=p, n=n: e.dma_start(out=wdb[p][:, :, :], in_=wd[n, :, :, :]), writes=[("wdb", p)], eng="gpsimd")
                for g in range(NG):
                    gs = slice(g * 512, (g + 1) * 512)
                    bk = (di * NG + g) % 8

                    def mm_d(e, p=p, gs=gs, bk=bk):
                        for fi, (fo, fs) in enumerate(fch):
                            ins = e.matmul(ps[bk][:, :], lhsT=wdb[p][0:fs, fi, :], rhs=hT[0:fs, fi, gs], start=(fi == 0), stop=(fi == NF - 1))
                        return ins

                    S.op("tensor", mm_d, reads=[("wdb", p)] + [("hT", fi, g) for fi in range(NF)], writes=[("ps", bk)])
                    r = ei % 2
                    ei += 1
                    S.op("vector", lambda e, r=r, gs=gs, bk=bk: e.tensor_tensor(out=yo[r][:, :], in0=ps[bk][:, :], in1=comb_bc[:, gs], op=ALU.mult),
                         reads=[("ps", bk), "comb_bc"], writes=[("yo", r)])
                    S.dma(lambda e, r=r, n=n, t0=t0, g=g: e.dma_start(out=yT[n * 128:(n + 1) * 128, t0 + g * 512:t0 + (g + 1) * 512], in_=yo[r][:, :]),
                          reads=[("yo", r)], writes=[("yT", n, t0, g)])
                di += 1
        S.emit(nc, st)
    return nc


def build_combine(D, TL, NP):
    from contextlib import ExitStack
    KD = D // 128
    nc = bass.Bass("TRN2", target_bir_lowering=False)
    x1T = nc.dram_tensor("x1T", [D, TL], F32, kind="ExternalInput").ap()
    parts = nc.dram_tensor("parts", [NP * D, TL], F32, kind="ExternalInput").ap()
    gb = nc.dram_tensor("gb", [128, 2 * KD], F32, kind="ExternalInput").ap()
    xo = nc.dram_tensor("xo", [D, TL], F32, kind="ExternalOutput").ap()
    S = Sched()
    with ExitStack() as st:
        sb = lambda name, shape, dt: st.enter_context(nc.sbuf_tensor(name, shape, dt))
        v = sb("v", [128, KD, TL], F32)
        o = sb("o", [128, KD, TL], F32)
        pt = [sb("pt%d" % i, [128, TL], F32) for i in range(2)]
        gbt = sb("gbt", [128, 2 * KD], F32)
        ones_b = sb("ones_b", [128, 128], BF16)
        scr = {"b1": sb("b1", [128, 512], BF16), "b2": sb("b2", [128, 512], BF16),
               "mean": sb("mean", [128, 512], F32), "rstd": sb("rstd", [128, 512], F32), "t": sb("t", [128, 512], F32)}
        ps = [st.enter_context(nc.psum_tensor("ps%d" % i, [128, 512], F32)) for i in range(2)]
        S.op("vector", lambda e: e.memset(ones_b[:, :], 1.0), writes=[("ln", "ones")])
        S.dma(lambda e: e.dma_start(out=gbt[:, :], in_=gb[:, :]), writes=[("ln", "gb")])
        S.dma(lambda e: e.dma_start(out=v[:, :, :], in_=x1T.rearrange("(k p) t -> p k t", p=128)),
              writes=[("ln", "v", k) for k in range(KD)])
        pi = 0
        for k in range(KD):
            S.op("scalar", lambda e, k=k: e.activation(out=v[:, k, :], in_=v[:, k, :], func=AF.Copy, scale=float(ALPHA)),
                 reads=[("ln", "v", k)], writes=[("ln", "v", k)])
            for ei in range(NP):
                p = pi % 2
                pi += 1
                S.dma(lambda e, p=p, ei=ei, k=k: e.dma_start(out=pt[p][:, :], in_=parts[ei * D + k * 128: ei * D + (k + 1) * 128, :]),
                      writes=[("pt", p)])
                S.op("vector", lambda e, p=p, k=k: e.tensor_tensor(out=v[:, k, :], in0=v[:, k, :], in1=pt[p][:, :], op=ALU.add),
                     reads=[("pt", p), ("ln", "v", k)], writes=[("ln", "v", k)])
        _ln_feature_major(S, nc, v, KD, TL, gbt, 0, KD, o, None, scr, ps, ones_b, "ln")
        S.dma(lambda e: e.dma_start(out=xo.rearrange("(k p) t -> p k t", p=128), in_=o[:, :, :]),
              reads=[("ln", "of", k) for k in range(KD)], writes=["xo"])
        S.emit(nc, st)
    return nc


def mix_dims(D, H, E):
    CC = D // 2
    A = H * 128
    off = {"a": 0, "g": CC, "q": 2 * CC, "k": 2 * CC + A, "v": 2 * CC + 2 * A, "f": 2 * CC + 3 * A}
    off["gc"] = off["f"] + H
    off["ga"] = off["gc"] + D
    off["end"] = off["ga"] + D
    return CC, A, off


def _tile_cols(w):
    K_, N_ = w.shape
    return np.ascontiguousarray(w.reshape(K_ // 128, 128, N_ // 128, 128).transpose(2, 1, 0, 3))


def mix_weight_layout(wl, off):
    D = wl.shape[0]
    main = np.concatenate([wl[:, :off["f"]], wl[:, off["gc"]:]], axis=1)
    wf = wl[:, off["f"]:off["gc"]]
    return _tile_cols(main), np.ascontiguousarray(wf.reshape(D // 128, 128, -1).transpose(1, 0, 2))


def build_mix(D, TL, H, E, GS=256):
    from contextlib import ExitStack
    CC, A, off = mix_dims(D, H, E)
    KD, KC = D // 128, CC // 128
    NGO = TL // GS
    NGA = 2 * NGO
    NKT = 2 * TL // 128
    TPG = GS // 128
    INC = off["end"]
    NB = (off["f"] // 128) + 2 * KD
    VO = {"bm": 0, "cb": NB, "cg": NB + KC, "cbeta": NB + 2 * KC, "bco": NB + 3 * KC,
          "l1g": NB + 3 * KC + KD, "l1b": NB + 3 * KC + 2 * KD}
    NV = NB + 3 * KC + 3 * KD
    QS = float(HEAD_DIM) ** -0.5
    BIG = 1.0e30

    nc = bass.Bass("TRN2", target_bir_lowering=False)
    dt_in = lambda n, s, d=F32: nc.dram_tensor(n, s, d, kind="ExternalInput").ap()
    x_own = dt_in("x_own", [D, TL])
    x_pre = dt_in("x_pre", [D, TL])
    w_in = dt_in("w_in", [NB, 128, KD, 128])
    w_f = dt_in("w_f", [128, KD, H])
    w_co = dt_in("w_co", [KD, 128, KC, 128])
    w_ao = dt_in("w_ao", [KD, 128, H, 128])
    w_o = dt_in("w_o", [KD, 128, KD, 128])
    vec = dt_in("vec", [128, NV])
    bf = dt_in("bf", [H, 1])
    cw = dt_in("cw", [128, KC * CONV_K])
    w_r = dt_in("w_r", [D, E])
    b_r = dt_in("b_r", [1, E])
    flag = dt_in("flag", [128, 1])
    x1T = nc.dram_tensor("x1T", [D, TL], F32, kind="ExternalOutput").ap()
    x1Tb = nc.dram_tensor("x1Tb", [D, TL], BF16, kind="ExternalOutput").ap()
    comb = nc.dram_tensor("comb", [TL, E], F32, kind="ExternalOutput").ap()

    S = Sched()
    with ExitStack() as st:
        sb = lambda name, shape, dt: st.enter_context(nc.sbuf_tensor(name, shape, dt))
        pst = lambda name, shape, dt: st.enter_context(nc.psum_tensor(name, shape, dt))
        ones_b = sb("ones_b", [128, 128], BF16)
        ones_f = sb("ones_f", [128, 128], F32)
        ident_b = sb("ident_b", [128, 128], BF16)
        ident_f = sb("ident_f", [H, H], F32)
        sel = sb("sel", [H, H, 128], F32)
        dmask = [sb("dmask%d" % i, [128, GS], BF16) for i in range(TPG)]
        vect = sb("vect", [128, NV], F32)
        bqs = sb("bqs", [128, H], F32)
        bft = sb("bft", [H, 1], F32)
        cwt = sb("cwt", [128, KC * CONV_K], F32)
        wrt = sb("wrt", [128, KD, E], F32)
        brt = sb("brt", [1, E], F32)
        flg = sb("flg", [128, 1], F32)
        mb = sb("mb", [128, 1], F32)
        kT = sb("kT", [128, H, 2 * TL], BF16)
        V = sb("V", [128, NKT, A], BF16)
        fT = sb("fT", [H, 2 * TL], F32)
        f2 = sb("f2", [H, 2 * TL], F32)
        cumT = sb("cumT", [H, 2 * TL], F32)
        onesH = sb("onesH", [H, 2 * TL], F32)
        nbias = sb("nbias", [128, NKT, H], F32)
        uhalo = sb("uhalo", [128, KC, 32], F32)
        utail = sb("utail", [128, KC, 32], F32)
        xs = [sb("xs%d" % i, [128, GS], F32) for i in range(2)]
        xb = sb("xb", [128, KD, GS], BF16)
        qT = sb("qT", [128, H, GS], BF16)
        UW = GS + 32
        big = sb("big", [128, max(KD * GS, KC * (UW + GS))], F32)
        u = big[:, 0:KC * UW].rearrange("p (c t) -> p c t", t=UW)
        y = big[:, KC * UW:KC * UW + KC * GS].rearrange("p (c t) -> p c t", t=GS)
        v = big[:, 0:KD * GS].rearrange("p (c t) -> p c t", t=GS)
        z = sb("z", [128, KC, GS], BF16)
        OT = sb("OT", [128, H, GS], BF16)
        mT = sb("mT", [128, KD, GS], BF16)
        vtmp = sb("vtmp", [128, GS], BF16)
        asb = sb("asb", [128, GS], F32)
        sgb = sb("sgb", [128, GS], F32)
        gcb = sb("gcb", [128, GS], F32)
        gab = sb("gab", [128, GS], F32)
        ycb = sb("ycb", [128, GS], F32)
        PT = [sb("PT%d" % i, [128, GS], BF16) for i in range(2)]
        rden = sb("rden", [128, GS], F32)
        stb = [sb("stb%d" % i, [128, GS], BF16) for i in range(2)]
        NWB = 4
        wb = [sb("wb%d" % i, [128, KD, 128], BF16) for i in range(NWB)]
        wfb = sb("wfb", [128, KD, H], BF16)
        scr = {"b1": sb("b1", [128, 512], BF16), "b2": sb("b2", [128, 512], BF16),
               "mean": sb("mean", [128, 512], F32), "rstd": sb("rstd", [128, 512], F32), "t": sb("t", [128, 512], F32)}
        rt = {n: sb("rt_" + n, [128, E], F32) for n in ("lg", "eq1", "lg2", "eq2", "c1", "cm")}
        r1 = {n: sb("r1_" + n, [128, 1], F32) for n in ("m1", "m2", "d", "e", "w1", "w2")}
        pp = [pst("pp%d" % i, [128, 512], F32) for i in range(2)]
        psS = [pst("psS%d" % i, [128, 512], F32) for i in range(2)]
        po = pst("po", [128, GS], F32)
        pd = pst("pd", [128, GS], F32)
        pvt = pst("pvt", [128, 128], BF16)

        S.op("vector", lambda e: e.memset(ones_b[:, :], 1.0), writes=["ones_b", ("ln1", "ones"), ("cln", "ones")])
        S.op("vector", lambda e: e.memset(ones_f[:, :], 1.0), writes=["ones_f"])
        S.op("vector", lambda e: e.memset(onesH[:, :], 1.0), writes=["onesH"])
        S.op("gpsimd", lambda e: e.affine_select(out=ident_b[:, :], in_=ones_b[:, :], pattern=[[-1, 128]], compare_op=ALU.is_equal,
                                                 fill=0.0, base=0, channel_multiplier=1),
             reads=["ones_b"], writes=["ident_b"])
        S.op("gpsimd", lambda e: e.affine_select(out=ident_f[:, :], in_=ones_f[0:H, 0:H], pattern=[[-1, H]], compare_op=ALU.is_equal,
                                                 fill=0.0, base=0, channel_multiplier=1),
             reads=["ones_f"], writes=["ident_f"])
        for h in range(H):
            S.op("gpsimd", lambda e, h=h: e.affine_select(out=sel[:, h, :], in_=ones_f[0:H, :], pattern=[[0, 128]], compare_op=ALU.is_equal,
                                                          fill=0.0, base=-h, channel_multiplier=1),
                 reads=["ones_f"], writes=["sel"])
        ones_gs = sb("ones_gs", [128, GS], BF16)
        S.op("vector", lambda e: e.memset(ones_gs[:, :], 1.0), writes=["ones_gs"])
        for i in range(TPG):
            S.op("gpsimd", lambda e, i=i: e.affine_select(out=dmask[i][:, :], in_=ones_gs[:, :], pattern=[[1, GS]],
                                                          compare_op=ALU.is_ge, fill=0.0, base=-128 * i, channel_multiplier=-1),
                 reads=["ones_gs"], writes=[("dmask", i)])
        S.dma(lambda e: e.dma_start(out=vect[:, :], in_=vec[:, :]), writes=["vect", ("ln1", "gb"), ("cln", "gb")])
        S.dma(lambda e: e.dma_start(out=bft[:, :], in_=bf[:, :]), writes=["bft"])
        S.dma(lambda e: e.dma_start(out=cwt[:, :], in_=cw[:, :]), writes=["cwt"])
        S.dma(lambda e: e.dma_start(out=wrt[:, :, :], in_=w_r.rearrange("(k p) e -> p k e", p=128)), writes=["wrt"])
        S.dma(lambda e: e.dma_start(out=brt[:, :], in_=b_r[:, :]), writes=["brt"])
        S.dma(lambda e: e.dma_start(out=flg[:, :], in_=flag[:, :]), writes=["flg"])
        S.op("vector", lambda e: e.tensor_scalar(out=mb[:, :], in0=flg[:, :], scalar1=BIG, scalar2=-BIG, op0=ALU.mult, op1=ALU.add),
             reads=["flg"], writes=["mb"])
        qb0 = VO["bm"] + off["q"] // 128
        S.op("vector", lambda e: e.tensor_scalar(out=bqs[:, :], in0=vect[:, qb0:qb0 + H], scalar1=QS, scalar2=None, op0=ALU.mult),
             reads=["vect"], writes=["bqs"])

        wctr = [0]

        def proj(Wt, kch, size, act, tsl, n, out_ps, act_key, ps_key):
            p = wctr[0] % NWB
            wctr[0] += 1
            S.dma(lambda e: e.dma_start(out=wb[p][:, 0:kch, :], in_=Wt), writes=[("wb", p)], eng="gpsimd")

            def mm(e):
                for k in range(kch):
                    ins = e.matmul(out_ps[0:size, 0:n], lhsT=wb[p][:, k, 0:size], rhs=act[:, k, tsl], start=(k == 0), stop=(k == kch - 1))
                return ins

            S.op("tensor", mm, reads=[("wb", p)] + list(act_key), writes=[ps_key])

        CH = lambda name, j: w_in[off[name] // 128 + j, :, :, :]
        GCH = lambda which, j: w_in[off["f"] // 128 + which * KD + j, :, :, :]
        S.dma(lambda e: e.dma_start(out=wfb[:, :, :], in_=w_f[:, :, :]), writes=["wfb"], eng="gpsimd")

        def load_xb(src, g):
            for k in range(KD):
                q = k % 2
                S.dma(lambda e, k=k, q=q: e.dma_start(out=xs[q][:, :], in_=src[k * 128:(k + 1) * 128, g * GS:(g + 1) * GS]),
                      writes=[("xs", q)])
                S.op("vector", lambda e, k=k, q=q: e.tensor_copy(out=xb[:, k, :], in_=xs[q][:, :]),
                     reads=[("xs", q)], writes=["xb"])

        ppc = [0]

        def next_pp():
            i = ppc[0] % 2
            ppc[0] += 1
            return pp[i], ("pp", i)

        bcol = lambda name, j: vect[:, VO["bm"] + off[name] // 128 + j: VO["bm"] + off[name] // 128 + j + 1]
        gcol = lambda which, j: vect[:, VO["bm"] + off["f"] // 128 + which * KD + j: VO["bm"] + off["f"] // 128 + which * KD + j + 1]
        full = slice(0, GS)

        for t in range(NGA):
            src, g = (x_pre, t) if t < NGO else (x_own, t - NGO)
            load_xb(src, g)
            tcol = slice(t * GS, (t + 1) * GS)
            for h in range(H):
                ps_, pk = next_pp()
                proj(CH("k", h), KD, 128, xb, full, GS, ps_, ["xb"], pk)
                S.op("scalar", lambda e, h=h, ps_=ps_, tcol=tcol: e.activation(out=kT[:, h, tcol], in_=ps_[:, 0:GS], func=AF.Identity,
                                                                            bias=bcol("k", h), scale=1.0),
                     reads=[pk, "vect"], writes=[("kT", h)])
            for h in range(H):
                ps_, pk = next_pp()
                proj(CH("v", h), KD, 128, xb, full, GS, ps_, ["xb"], pk)
                S.op("scalar", lambda e, h=h, ps_=ps_: e.activation(out=vtmp[:, :], in_=ps_[:, 0:GS], func=AF.Identity,
                                                                   bias=bcol("v", h), scale=1.0),
                     reads=[pk, "vect"], writes=["vtmp"])
                for j in range(TPG):
                    kt = t * TPG + j
                    S.op("tensor", lambda e, j=j: e.transpose(pvt[:, :], vtmp[:, j * 128:(j + 1) * 128], ident_b[:, :]),
                         reads=["vtmp", "ident_b"], writes=["pvt"])
                    S.op("vector", lambda e, kt=kt, h=h: e.tensor_copy(out=V[:, kt, h * 128:(h + 1) * 128], in_=pvt[:, :]),
                         reads=["pvt"], writes=[("V", kt)])
            ps_, pk = next_pp()

            def mm_f(e, ps_=ps_):
                for k in range(KD):
                    ins = e.matmul(ps_[0:H, 0:GS], lhsT=wfb[:, k, :], rhs=xb[:, k, :], start=(k == 0), stop=(k == KD - 1))
                return ins

            S.op("tensor", mm_f, reads=["wfb", "xb"], writes=[pk])
            S.op("scalar", lambda e, ps_=ps_, tcol=tcol: e.activation(out=fT[:, tcol], in_=ps_[0:H, 0:GS], func=AF.Identity,
                                                                   bias=bft[:, 0:1], scale=1.0),
                 reads=[pk, "bft"], writes=["fT"])
            if t == NGO - 1:
                hs = slice(GS - 32, GS)
                for c in range(KC):
                    ps_, pk = next_pp()
                    proj(CH("a", c), KD, 128, xb, hs, 32, ps_, ["xb"], pk)
                    S.op("scalar", lambda e, c=c, ps_=ps_: e.activation(out=asb[:, 0:32], in_=ps_[:, 0:32], func=AF.Identity,
                                                                       bias=bcol("a", c), scale=1.0),
                         reads=[pk, "vect"], writes=["asb"])
                    ps2, pk2 = next_pp()
                    proj(CH("g", c), KD, 128, xb, hs, 32, ps2, ["xb"], pk2)
                    S.op("scalar", lambda e, c=c, ps2=ps2: e.activation(out=sgb[:, 0:32], in_=ps2[:, 0:32], func=AF.Sigmoid,
                                                                       bias=bcol("g", c), scale=1.0),
                         reads=[pk2, "vect"], writes=["sgb"])
                    S.op("vector", lambda e, c=c: e.tensor_tensor(out=uhalo[:, c, :], in0=asb[:, 0:32], in1=sgb[:, 0:32], op=ALU.mult),
                         reads=["asb", "sgb"], writes=["uhalo"])
                    S.op("vector", lambda e, c=c: e.tensor_scalar(out=uhalo[:, c, :], in0=uhalo[:, c, :], scalar1=flg[:, 0:1], scalar2=None, op0=ALU.mult),
                         reads=["uhalo", "flg"], writes=["uhalo"])

        S.op("vector", lambda e: e.tensor_scalar(out=f2[:, :], in0=fT[:, :], scalar1=-1.0, scalar2=None, op0=ALU.mult),
             reads=["fT"], writes=["f2"])
        S.op("vector", lambda e: e.tensor_tensor(out=f2[:, :], in0=f2[:, :], in1=fT[:, :], op=ALU.max),
             reads=["fT", "f2"], writes=["f2"])
        S.op("scalar", lambda e: e.activation(out=f2[:, :], in_=f2[:, :], func=AF.Exp, scale=-1.0), reads=["f2"], writes=["f2"])
        S.op("scalar", lambda e: e.activation(out=f2[:, :], in_=f2[:, :], func=AF.Ln, bias=1.0, scale=1.0), reads=["f2"], writes=["f2"])
        S.op("vector", lambda e: e.tensor_scalar(out=fT[:, :], in0=fT[:, :], scalar1=0.0, scalar2=None, op0=ALU.min),
             reads=["fT"], writes=["fT"])
        S.op("vector", lambda e: e.tensor_tensor(out=fT[:, :], in0=fT[:, :], in1=f2[:, :], op=ALU.subtract),
             reads=["fT", "f2"], writes=["fT"])
        S.op("vector", lambda e: e.tensor_tensor_scan(out=cumT[:, :], data0=onesH[:, :], data1=fT[:, :], initial=0.0,
                                                      op0=ALU.mult, op1=ALU.add),
             reads=["fT", "onesH"], writes=["cumT"])
        for kt in range(NKT):
            ps_, pk = next_pp()
            S.op("tensor", lambda e, kt=kt, ps_=ps_: e.transpose(ps_[:, 0:H], cumT[:, kt * 128:(kt + 1) * 128], ident_f[:, :]),
                 reads=["cumT", "ident_f"], writes=[pk])
            if kt < NKT // 2:
                S.op("vector", lambda e, kt=kt, ps_=ps_: e.tensor_scalar(out=nbias[:, kt, :], in0=ps_[:, 0:H], scalar1=-1.0, scalar2=mb[:, 0:1],
                                                                        op0=ALU.mult, op1=ALU.add),
                     reads=[pk, "mb"], writes=["nbias"])
            else:
                S.op("vector", lambda e, kt=kt, ps_=ps_: e.tensor_scalar(out=nbias[:, kt, :], in0=ps_[:, 0:H], scalar1=-1.0, scalar2=None, op0=ALU.mult),
                     reads=[pk], writes=["nbias"])

        ukeys = [("u", c) for c in range(KC)] + [("y", c) for c in range(KC)] + [("cln", "v", c) for c in range(KC)] + [("cln", "of", c) for c in range(KC)]
        vkeys = [("ln1", "v", k) for k in range(KD)] + [("ln1", "of", k) for k in range(KD)]
        sti = [0]
        for gi in range(NGO):
            load_xb(x_own, gi)
            gq = slice(TL + gi * GS, TL + (gi + 1) * GS)
            for c in range(KC):
                ps_, pk = next_pp()
                proj(CH("a", c), KD, 128, xb, full, GS, ps_, ["xb"], pk)
                S.op("scalar", lambda e, c=c, ps_=ps_: e.activation(out=asb[:, :], in_=ps_[:, 0:GS], func=AF.Identity, bias=bcol("a", c), scale=1.0),
                     reads=[pk, "vect"], writes=["asb"])
                ps2, pk2 = next_pp()
                proj(CH("g", c), KD, 128, xb, full, GS, ps2, ["xb"], pk2)
                S.op("scalar", lambda e, c=c, ps2=ps2: e.activation(out=sgb[:, :], in_=ps2[:, 0:GS], func=AF.Sigmoid, bias=bcol("g", c), scale=1.0),
                     reads=[pk2, "vect"], writes=["sgb"])
                S.op("vector", lambda e, c=c: e.tensor_tensor(out=u[:, c, 32:32 + GS], in0=asb[:, :], in1=sgb[:, :], op=ALU.mult),
                     reads=["asb", "sgb"], writes=[("u", c)] + (vkeys if c == 0 else []))
                hsrc = uhalo if gi == 0 else utail
                S.op("vector", lambda e, c=c, hsrc=hsrc: e.tensor_copy(out=u[:, c, 0:32], in_=hsrc[:, c, :]),
                     reads=["uhalo", "utail"], writes=[("u", c)])
                ceng = "vector"
                for j in range(CONV_K):
                    wcol = cwt[:, c * CONV_K + j: c * CONV_K + j + 1]
                    src = u[:, c, 2 + j: 2 + j + GS]
                    if j == 0:
                        S.op(ceng, lambda e, c=c, wcol=wcol, src=src: e.tensor_scalar(out=y[:, c, :], in0=src, scalar1=wcol,
                                                                                    scalar2=vect[:, VO["cb"] + c: VO["cb"] + c + 1], op0=ALU.mult, op1=ALU.add),
                             reads=[("u", c), "cwt", "vect"], writes=[("y", c), ("cln", "v", c)])
                    else:
                        S.op(ceng, lambda e, c=c, wcol=wcol, src=src: e.scalar_tensor_tensor(out=y[:, c, :], in0=src, scalar=wcol, in1=y[:, c, :],
                                                                                           op0=ALU.mult, op1=ALU.add),
                             reads=[("u", c), "cwt", ("y", c)], writes=[("y", c), ("cln", "v", c)])
                S.op("vector", lambda e, c=c: e.tensor_copy(out=utail[:, c, :], in_=u[:, c, GS:GS + 32]),
                     reads=[("u", c)], writes=["utail"])
            _ln_feature_major(S, nc, y, KC, GS, vect, VO["cg"], VO["cbeta"], y, None, scr, psS, ones_b, "cln", ps_keys=("psS0", "psS1"))
            for c in range(KC):
                S.op("scalar", lambda e, c=c: e.activation(out=z[:, c, :], in_=y[:, c, :], func=AF.Silu),
                     reads=[("cln", "of", c)], writes=[("z", c)])
            for h in range(H):
                ps_, pk = next_pp()
                proj(CH("q", h), KD, 128, xb, full, GS, ps_, ["xb"], pk)
                S.op("scalar", lambda e, h=h, ps_=ps_: e.activation(out=qT[:, h, :], in_=ps_[:, 0:GS], func=AF.Identity, bias=bqs[:, h:h + 1], scale=QS),
                     reads=[pk, "bqs"], writes=[("qT", h)])
            nkt_g = NKT // 2 + (gi + 1) * TPG
            for h in range(H):
                for kt in range(nkt_g):
                    sp = kt % 2
                    S.op("tensor", lambda e, h=h, kt=kt, sp=sp: e.matmul(psS[sp][:, 0:GS], lhsT=kT[:, h, kt * 128:(kt + 1) * 128], rhs=qT[:, h, :],
                                                                        start=True, stop=False),
                         reads=[("kT", h), ("qT", h)], writes=["psS%d" % sp])
                    S.op("tensor", lambda e, h=h, sp=sp, gq=gq: e.matmul(psS[sp][:, 0:GS], lhsT=sel[:, h, :], rhs=cumT[:, gq], start=False, stop=True),
                         reads=["sel", "cumT"], writes=["psS%d" % sp])
                    S.op("scalar", lambda e, h=h, kt=kt, sp=sp: e.activation(out=PT[sp][:, :], in_=psS[sp][:, 0:GS], func=AF.Exp,
                                                                            bias=nbias[:, kt, h:h + 1], scale=1.0),
                         reads=["psS%d" % sp, "nbias"], writes=[("PT", sp)])
                    dj = kt - (NKT // 2 + gi * TPG)
                    if dj >= 0:
                        S.op("vector", lambda e, sp=sp, dj=dj: e.tensor_tensor(out=PT[sp][:, :], in0=PT[sp][:, :], in1=dmask[dj][:, :], op=ALU.mult),
                             reads=[("PT", sp), ("dmask", dj)], writes=[("PT", sp)])
                    S.op("tensor", lambda e, h=h, kt=kt, sp=sp, nkt_g=nkt_g: e.matmul(po[:, 0:GS], lhsT=V[:, kt, h * 128:(h + 1) * 128], rhs=PT[sp][:, :],
                                                                        start=(kt == 0), stop=(kt == nkt_g - 1)),
                         reads=[("V", kt), ("PT", sp)], writes=["po"])
                    S.op("tensor", lambda e, kt=kt, sp=sp, nkt_g=nkt_g: e.matmul(pd[:, 0:GS], lhsT=ones_b[:, :], rhs=PT[sp][:, :],
                                                                   start=(kt == 0), stop=(kt == nkt_g - 1)),
                         reads=["ones_b", ("PT", sp)], writes=["pd"])
                S.op("vector", lambda e: e.reciprocal(out=rden[:, :], in_=pd[:, 0:GS]), reads=["pd"], writes=["rden"])
                S.op("vector", lambda e, h=h: e.tensor_tensor(out=OT[:, h, :], in0=po[:, 0:GS], in1=rden[:, :], op=ALU.mult),
                     reads=["po", "rden"], writes=[("OT", h)])
            for n in range(KD):
                ps_, pk = next_pp()
                proj(GCH(0, n), KD, 128, xb, full, GS, ps_, ["xb"], pk)
                S.op("scalar", lambda e, n=n, ps_=ps_: e.activation(out=gcb[:, :], in_=ps_[:, 0:GS], func=AF.Sigmoid, bias=gcol(0, n), scale=1.0),
                     reads=[pk, "vect"], writes=["gcb"])
                ps_, pk = next_pp()
                proj(GCH(1, n), KD, 128, xb, full, GS, ps_, ["xb"], pk)
                S.op("scalar", lambda e, n=n, ps_=ps_: e.activation(out=gab[:, :], in_=ps_[:, 0:GS], func=AF.Sigmoid, bias=gcol(1, n), scale=1.0),
                     reads=[pk, "vect"], writes=["gab"])
                ps_, pk = next_pp()
                proj(w_co[n, :, :, :], KC, 128, z, full, GS, ps_, [("z", c) for c in range(KC)], pk)
                S.op("scalar", lambda e, n=n, ps_=ps_: e.activation(out=ycb[:, :], in_=ps_[:, 0:GS], func=AF.Identity,
                                                                   bias=vect[:, VO["bco"] + n: VO["bco"] + n + 1], scale=1.0),
                     reads=[pk, "vect"], writes=["ycb"])
                S.op("vector", lambda e: e.tensor_tensor(out=gcb[:, :], in0=gcb[:, :], in1=ycb[:, :], op=ALU.mult),
                     reads=["gcb", "ycb"], writes=["gcb"])
                ps_, pk = next_pp()
                proj(w_ao[n, :, :, :], H, 128, OT, full, GS, ps_, [("OT", h) for h in range(H)], pk)
                S.op("vector", lambda e, ps_=ps_: e.tensor_tensor(out=gab[:, :], in0=gab[:, :], in1=ps_[:, 0:GS], op=ALU.mult),
                     reads=["gab", pk], writes=["gab"])
                S.op("vector", lambda e, n=n: e.tensor_tensor(out=mT[:, n, :], in0=gcb[:, :], in1=gab[:, :], op=ALU.add),
                     reads=["gcb", "gab"], writes=[("mT", n)])
            for n in range(KD):
                ps_, pk = next_pp()
                proj(w_o[n, :, :, :], KD, 128, mT, full, GS, ps_, [("mT", k) for k in range(KD)], pk)
                q = n % 2
                S.dma(lambda e, n=n, q=q, gi=gi: e.dma_start(out=xs[q][:, :], in_=x_own[n * 128:(n + 1) * 128, gi * GS:(gi + 1) * GS]),
                      writes=[("xs", q)])
                S.op("vector", lambda e, n=n, q=q, ps_=ps_: e.scalar_tensor_tensor(out=v[:, n, :], in0=xs[q][:, :], scalar=float(ALPHA), in1=ps_[:, 0:GS],
                                                                                 op0=ALU.mult, op1=ALU.add),
                     reads=[("xs", q), pk], writes=[("ln1", "v", n)] + (ukeys if n == 0 else []))
            _ln_feature_major(S, nc, v, KD, GS, vect, VO["l1g"], VO["l1b"], v, None, scr, psS, ones_b, "ln1", ps_keys=("psS0", "psS1"))
            gsl = slice(gi * GS, (gi + 1) * GS)
            for n in range(KD):
                S.dma(lambda e, n=n, gsl=gsl: e.dma_start(out=x1T[n * 128:(n + 1) * 128, gsl], in_=v[:, n, :]),
                      reads=[("ln1", "of", n)], writes=[("x1T", n, gi)])
                q = sti[0] % 2
                sti[0] += 1
                S.op("scalar", lambda e, n=n, q=q: e.activation(out=stb[q][:, :], in_=v[:, n, :], func=AF.Copy),
                     reads=[("ln1", "of", n)], writes=[("stb", q)])
                S.dma(lambda e, n=n, q=q, gsl=gsl: e.dma_start(out=x1Tb[n * 128:(n + 1) * 128, gsl], in_=stb[q][:, :]),
                      reads=[("stb", q)], writes=[("x1Tb", n, gi)])
            for j in range(TPG):
                ps_, pk = next_pp()
                for k in range(KD):
                    S.op("tensor", lambda e, k=k, j=j, ps_=ps_: e.matmul(ps_[:, 0:E], lhsT=v[:, k, j * 128:(j + 1) * 128], rhs=wrt[:, k, :],
                                                                        start=(k == 0), stop=False),
                         reads=[("ln1", "of", k), "wrt"], writes=[pk])
                S.op("tensor", lambda e, ps_=ps_: e.matmul(ps_[:, 0:E], lhsT=ones_f[0:1, :], rhs=brt[:, :], start=False, stop=True),
                     reads=["ones_f", "brt"], writes=[pk])
                R, r = rt, r1
                S.op("vector", lambda e, ps_=ps_: e.tensor_copy(out=R["lg"][:, :], in_=ps_[:, 0:E]), reads=[pk], writes=["r_lg"])
                S.op("vector", lambda e: e.reduce_max(out=r["m1"][:, :], in_=R["lg"][:, :], axis=mybir.AxisListType.X), reads=["r_lg"], writes=["r_m1"])
                S.op("vector", lambda e: e.tensor_scalar(out=R["eq1"][:, :], in0=R["lg"][:, :], scalar1=r["m1"][:, 0:1], scalar2=None, op0=ALU.is_equal),
                     reads=["r_lg", "r_m1"], writes=["r_eq1"])
                S.op("vector", lambda e: e.scalar_tensor_tensor(out=R["lg2"][:, :], in0=R["eq1"][:, :], scalar=-BIG, in1=R["lg"][:, :], op0=ALU.mult, op1=ALU.add),
                     reads=["r_eq1", "r_lg"], writes=["r_lg2"])
                S.op("vector", lambda e: e.reduce_max(out=r["m2"][:, :], in_=R["lg2"][:, :], axis=mybir.AxisListType.X), reads=["r_lg2"], writes=["r_m2"])
                S.op("vector", lambda e: e.tensor_scalar(out=R["eq2"][:, :], in0=R["lg2"][:, :], scalar1=r["m2"][:, 0:1], scalar2=None, op0=ALU.is_equal),
                     reads=["r_lg2", "r_m2"], writes=["r_eq2"])
                S.op("vector", lambda e: e.tensor_tensor(out=r["d"][:, :], in0=r["m2"][:, :], in1=r["m1"][:, :], op=ALU.subtract),
                     reads=["r_m1", "r_m2"], writes=["r_d"])
                S.op("scalar", lambda e: e.activation(out=r["e"][:, :], in_=r["d"][:, :], func=AF.Exp), reads=["r_d"], writes=["r_e"])
                S.op("vector", lambda e: e.tensor_scalar(out=r["w1"][:, :], in0=r["e"][:, :], scalar1=1.0, scalar2=None, op0=ALU.add),
                     reads=["r_e"], writes=["r_w1"])
                S.op("vector", lambda e: e.reciprocal(out=r["w1"][:, :], in_=r["w1"][:, :]), reads=["r_w1"], writes=["r_w1"])
                S.op("vector", lambda e: e.tensor_tensor(out=r["w2"][:, :], in0=r["e"][:, :], in1=r["w1"][:, :], op=ALU.mult),
                     reads=["r_e", "r_w1"], writes=["r_w2"])
                S.op("vector", lambda e: e.tensor_scalar(out=R["c1"][:, :], in0=R["eq1"][:, :], scalar1=r["w1"][:, 0:1], scalar2=None, op0=ALU.mult),
                     reads=["r_eq1", "r_w1"], writes=["r_c1"])
                S.op("vector", lambda e: e.scalar_tensor_tensor(out=R["cm"][:, :], in0=R["eq2"][:, :], scalar=r["w2"][:, 0:1], in1=R["c1"][:, :],
                                                                op0=ALU.mult, op1=ALU.add),
                     reads=["r_eq2", "r_w2", "r_c1"], writes=["r_cm"])
                t0 = gi * GS + j * 128
                S.dma(lambda e, t0=t0: e.dma_start(out=comb[t0:t0 + 128, :], in_=R["cm"][:, :]), reads=["r_cm"], writes=[("comb", t0)])
        S.emit(nc, st)
    return nc


_PROGS = {}


def _prog(key, fn):
    if key not in _PROGS:
        _PROGS[key] = fn()
    return _PROGS[key]


def _launch(nc, in_maps):
    res = run_bass_kernel_spmd(nc, in_maps, core_ids=list(range(NCORES)))
    return res.results


def _pk(v):
    return np.ascontiguousarray(np.asarray(v, np.float32).reshape(-1, 128).T)


def kernel(x, w_in, b_in, conv_w, conv_b, conv_ln_g, conv_ln_b, w_conv_out, b_conv_out,
           w_attn_out, w_o, ln1_g, ln1_b, ffn_wg, ffn_wu, ffn_wd, router_w, router_b,
           exp_wg, exp_wu, exp_wd, ln2_g, ln2_b):
    D, H, E, TL = D_MODEL, N_HEADS, N_EXPERTS, SEQ // 2
    T = BATCH * SEQ
    CC, A, off = mix_dims(D, H, E)
    KC = CC // 128
    x = np.asarray(x, np.float32)
    mix = _prog("mix", lambda: build_mix(D, TL, H, E))
    cmb = _prog("comb", lambda: build_combine(D, TL, NCORES))
    FS = D_FF // NCORES
    ffn_d = _prog("ffn_d", lambda: build_ffn(D, T, FS))
    ffn_e = _prog("ffn_e", lambda: build_ffn(D, T, D_FF))
    xcur = [np.ascontiguousarray(x[c // 2, (c % 2) * TL:(c % 2 + 1) * TL, :].T) for c in range(NCORES)]
    zeros_pre = np.zeros((D, TL), np.float32)
    ones_comb = np.ones((1, T), np.float32)
    for l in range(DEPTH):
        j = l // 2
        b = np.asarray(b_in[l], np.float32)
        bm = np.concatenate([b[:off["f"]], b[off["gc"]:]])
        vec = np.concatenate([_pk(bm), _pk(conv_b[l]), _pk(conv_ln_g[l]), _pk(conv_ln_b[l]), _pk(b_conv_out[l]),
                              _pk(ln1_g[l]), _pk(ln1_b[l])], axis=1)
        cw = np.ascontiguousarray(np.asarray(conv_w[l], np.float32).T.reshape(KC, 128, CONV_K).transpose(1, 0, 2).reshape(128, KC * CONV_K))
        bfv = np.ascontiguousarray(b[off["f"]:off["gc"]].reshape(H, 1))
        wl = np.asarray(w_in[l], np.float32)
        win_t, wf_t = mix_weight_layout(wl, off)
        wco_t = _tile_cols(np.asarray(w_conv_out[l], np.float32))
        wao_t = _tile_cols(np.asarray(w_attn_out[l], np.float32))
        wo_t = _tile_cols(np.asarray(w_o[l], np.float32))
        maps = []
        for c in range(NCORES):
            odd = c % 2 == 1
            maps.append({"x_own": xcur[c], "x_pre": xcur[c - 1] if odd else zeros_pre,
                         "w_in": win_t, "w_f": wf_t, "w_co": wco_t, "w_ao": wao_t, "w_o": wo_t,
                         "vec": vec, "bf": bfv, "cw": cw, "w_r": np.asarray(router_w[j], np.float32),
                         "b_r": np.asarray(router_b[j], np.float32).reshape(1, E),
                         "flag": np.full((128, 1), 1.0 if odd else 0.0, np.float32)})
        r = _launch(mix, maps)
        x1T = [np.asarray(r[c]["x1T"]) for c in range(NCORES)]
        xall = np.ascontiguousarray(np.concatenate([np.asarray(r[c]["x1Tb"]) for c in range(NCORES)], axis=1))
        maps = []
        if l % 2 == 0:
            for e in range(NCORES):
                g_, u_, d_ = ffn_weight_layout(ffn_wg[j][:, e * FS:(e + 1) * FS], ffn_wu[j][:, e * FS:(e + 1) * FS],
                                               ffn_wd[j][e * FS:(e + 1) * FS, :])
                maps.append({"xT": xall, "comb": ones_comb, "wg": g_, "wu": u_, "wd": d_})
            r = _launch(ffn_d, maps)
        else:
            call = np.concatenate([np.asarray(r[c]["comb"]) for c in range(NCORES)], axis=0)
            for e in range(NCORES):
                g_, u_, d_ = ffn_weight_layout(exp_wg[j][e], exp_wu[j][e], exp_wd[j][e])
                maps.append({"xT": xall, "comb": np.ascontiguousarray(call[:, e].reshape(1, T)), "wg": g_, "wu": u_, "wd": d_})
            r = _launch(ffn_e, maps)
        yT = [np.asarray(r[e]["yT"]) for e in range(NCORES)]
        gb = np.concatenate([_pk(ln2_g[l]), _pk(ln2_b[l])], axis=1)
        maps = []
        for c in range(NCORES):
            parts = np.ascontiguousarray(np.concatenate([yT[e][:, c * TL:(c + 1) * TL] for e in range(NCORES)], axis=0))
            maps.append({"x1T": x1T[c], "parts": parts, "gb": gb})
        r = _launch(cmb, maps)
        xcur = [np.asarray(r[c]["xo"]) for c in range(NCORES)]
    out = np.empty((BATCH, SEQ, D), np.float32)
    for c in range(NCORES):
        out[c // 2, (c % 2) * TL:(c % 2 + 1) * TL, :] = xcur[c].T
    return out
```
